# Optimizing a Trainium2 kernel written in Bass

```python
import functools
import numpy as np
import jax
import jax.numpy as jnp
from jax import lax

D_MODEL = 1024
BATCH = 2
SEQ = 8192
DEPTH = 1
DEC_BATCH = 128
DEC_SEQ = 1
PAST_LEN = 2048
PAGE_SIZE = 128

D_CONV = 1024
CONV_WIDTH = 3
N_HEADS = 8
HEAD_DIM = 128
N_KV_HEADS = 2
GROUP = N_HEADS // N_KV_HEADS
IDX_HEADS = 4
IDX_DIM = 64
TOP_K_MAX = 256
Q_BLOCK = 128
N_MEM = 256
MEM_HEADS = 4
MEM_HEAD_DIM = D_MODEL // MEM_HEADS
D_FF = -(-8 * D_MODEL // (3 * 256)) * 256
EPS = 1e-6
NEG_INF = -1e30
IDX_SCALE = (IDX_HEADS * IDX_DIM) ** -0.5
MIX_WIDTHS = (D_CONV, D_CONV, D_CONV, N_HEADS * HEAD_DIM, N_KV_HEADS * HEAD_DIM, N_KV_HEADS * HEAD_DIM,
              IDX_HEADS * IDX_DIM, IDX_DIM, IDX_HEADS, D_MODEL, D_MODEL)
MIX_IN = sum(MIX_WIDTHS)

kernel_name = "hybrid_conv_dsa_gated_decoder_step"


def rms_norm(x, g):
    xf = x.astype(jnp.float32)
    y = xf * lax.rsqrt(jnp.mean(xf * xf, axis=-1, keepdims=True) + EPS)
    return (y * g.astype(jnp.float32)).astype(x.dtype)


def gather_rows(src, idx):
    return jax.vmap(lambda s, i: s[i])(src, idx)


def short_conv(u, prev, conv_w):
    T = u.shape[1]
    upad = jnp.concatenate([prev.astype(u.dtype), u], axis=1)
    y = sum(upad[:, j:j + T] * conv_w[j] for j in range(CONV_WIDTH))
    return y, upad[:, T:]


def indexer_topk(iq, iw, ik, q_pos, top_k):
    s = jnp.einsum('bthd,bsd->bths', iq.astype(jnp.float32), ik.astype(jnp.float32))
    score = jnp.einsum('bths,bth->bts', jax.nn.relu(s), iw.astype(jnp.float32)) * IDX_SCALE
    key_pos = jnp.arange(ik.shape[1])
    causal = key_pos[None, None, :] <= q_pos[None, :, None]
    score = jnp.where(causal, score, NEG_INF)
    _, idx = lax.top_k(score, top_k)
    valid = idx <= q_pos[None, :, None]
    return idx, valid


def sparse_attend(q, ks, vs, valid):
    B, T = q.shape[:2]
    qg = q.reshape(B, T, N_KV_HEADS, GROUP, HEAD_DIM).astype(jnp.float32)
    s = jnp.einsum('btkgd,btjkd->btkgj', qg, ks.astype(jnp.float32)) * HEAD_DIM ** -0.5
    s = jnp.where(valid[:, :, None, None, :], s, NEG_INF)
    p = jax.nn.softmax(s, axis=-1)
    o = jnp.einsum('btkgj,btjkd->btkgd', p, vs.astype(jnp.float32))
    return o.reshape(B, T, N_HEADS * HEAD_DIM).astype(q.dtype)


def prompt_sparse_attention(q, k, v, iq, ik, iw):
    B, S = q.shape[:2]
    top_k = min(TOP_K_MAX, S // 4)
    n_blocks = S // Q_BLOCK

    def block(i):
        start = i * Q_BLOCK
        qb = lax.dynamic_slice_in_dim(q, start, Q_BLOCK, axis=1)
        iqb = lax.dynamic_slice_in_dim(iq, start, Q_BLOCK, axis=1)
        iwb = lax.dynamic_slice_in_dim(iw, start, Q_BLOCK, axis=1)
        pos = start + jnp.arange(Q_BLOCK)
        idx, valid = indexer_topk(iqb, iwb, ik, pos, top_k)
        ks = gather_rows(k, idx)
        vs = gather_rows(v, idx)
        return sparse_attend(qb, ks, vs, valid)

    out = lax.map(block, jnp.arange(n_blocks))
    return out.transpose(1, 0, 2, 3).reshape(B, S, N_HEADS * HEAD_DIM)


def sample_sparse_attention(q, k_new, v_new, iq, ik_new, iw, cache_k, cache_v, cache_ik, page_table):
    Bd, T = q.shape[:2]
    page_size = cache_k.shape[1]
    past = page_table.shape[1] * page_size
    top_k = min(TOP_K_MAX, (past + T) // 4)
    ik_past = cache_ik[page_table].reshape(Bd, past, IDX_DIM)
    ik_all = jnp.concatenate([ik_past.astype(ik_new.dtype), ik_new], axis=1)
    q_pos = past + jnp.arange(T)
    idx, valid = indexer_topk(iq, iw, ik_all, q_pos, top_k)
    in_past = (idx < past)[..., None, None]
    pidx = jnp.minimum(idx, past - 1)
    phys = gather_rows(page_table, pidx // page_size)
    slot = pidx % page_size
    nidx = jnp.clip(idx - past, 0, T - 1)
    ks = jnp.where(in_past, cache_k[phys, slot].astype(k_new.dtype), gather_rows(k_new, nidx))
    vs = jnp.where(in_past, cache_v[phys, slot].astype(v_new.dtype), gather_rows(v_new, nidx))
    return sparse_attend(q, ks, vs, valid)


def memory_kv(mem, g, w_mk, w_mv):
    B, M, _ = mem.shape
    m = rms_norm(mem, g)
    k = jnp.einsum('bmd,de->bme', m, w_mk).reshape(B, M, MEM_HEADS, MEM_HEAD_DIM)
    v = jnp.einsum('bmd,de->bme', m, w_mv).reshape(B, M, MEM_HEADS, MEM_HEAD_DIM)
    return k, v


def memory_attend(h, mem_k, mem_v, w_mq, w_mo):
    B, T, _ = h.shape
    q = jnp.einsum('btd,de->bte', h, w_mq).reshape(B, T, MEM_HEADS, MEM_HEAD_DIM)
    s = jnp.einsum('bthd,bmhd->bhtm', q.astype(jnp.float32), mem_k.astype(jnp.float32)) * MEM_HEAD_DIM ** -0.5
    p = jax.nn.softmax(s, axis=-1)
    o = jnp.einsum('bhtm,bmhd->bthd', p, mem_v.astype(jnp.float32)).reshape(B, T, D_MODEL).astype(h.dtype)
    return jnp.einsum('btd,de->bte', o, w_mo)


def trunk_layer(x, conv_prev, attend, mem_k, mem_v, lw):
    (g_mix, w_in, conv_w, w_conv_out, w_attn_out, w_o,
     g_mem, w_mq, w_mo, g_ffn, w_gate, w_up, w_down) = lw
    B, T, _ = x.shape
    h = rms_norm(x, g_mix)
    z = jnp.einsum('btd,de->bte', h, w_in)
    splits = [int(s) for s in np.cumsum(MIX_WIDTHS)[:-1]]
    (c_in, c_b, c_c, q, k, v, iq, ik, iw, gate_a, gate_b) = jnp.split(z, splits, axis=-1)
    conv_out, conv_state = short_conv(c_c * c_in, conv_prev, conv_w)
    out_a = jnp.einsum('btc,cd->btd', c_b * conv_out, w_conv_out)
    q = q.reshape(B, T, N_HEADS, HEAD_DIM)
    k = k.reshape(B, T, N_KV_HEADS, HEAD_DIM)
    v = v.reshape(B, T, N_KV_HEADS, HEAD_DIM)
    iq = iq.reshape(B, T, IDX_HEADS, IDX_DIM)
    attn = attend(q, k, v, iq, ik, iw)
    out_b = jnp.einsum('bte,ed->btd', attn, w_attn_out)
    merged = jax.nn.sigmoid(gate_a) * out_a + jax.nn.sigmoid(gate_b) * out_b
    x = x + jnp.einsum('btd,de->bte', merged, w_o)
    x = x + memory_attend(rms_norm(x, g_mem), mem_k, mem_v, w_mq, w_mo)
    h = rms_norm(x, g_ffn)
    f = jax.nn.silu(jnp.einsum('btd,df->btf', h, w_gate)) * jnp.einsum('btd,df->btf', h, w_up)
    x = x + jnp.einsum('btf,fd->btd', f, w_down)
    return x, conv_state, k, v, ik


def setup_inputs(seed: int = 0) -> dict:
    key = jax.random.key(seed)
    ks = jax.random.split(key, 32)
    n_pages = PAST_LEN // PAGE_SIZE
    n_used = DEC_BATCH * n_pages
    n_phys = (5 * n_used + 3) // 4
    f32 = jnp.float32

    def nrm(k, shape, scale=1.0):
        return jax.random.normal(k, shape, f32) * scale

    def gain(k, shape):
        return 1.0 + 0.05 * jax.random.normal(k, shape, f32)

    page_table = jax.random.permutation(ks[0], n_phys)[:n_used].reshape(DEC_BATCH, n_pages).astype(jnp.int32)
    return {
        "x_prompt": nrm(ks[1], (BATCH, SEQ, D_MODEL)),
        "x_sample": nrm(ks[2], (DEC_BATCH, DEC_SEQ, D_MODEL)),
        "mem_prompt": nrm(ks[3], (BATCH, N_MEM, D_MODEL)),
        "cache_k": nrm(ks[4], (DEPTH, n_phys, PAGE_SIZE, N_KV_HEADS, HEAD_DIM)),
        "cache_v": nrm(ks[5], (DEPTH, n_phys, PAGE_SIZE, N_KV_HEADS, HEAD_DIM)),
        "cache_idx_k": nrm(ks[6], (DEPTH, n_phys, PAGE_SIZE, IDX_DIM)),
        "cache_mem_k": nrm(ks[7], (DEPTH, DEC_BATCH, N_MEM, MEM_HEADS, MEM_HEAD_DIM)),
        "cache_mem_v": nrm(ks[8], (DEPTH, DEC_BATCH, N_MEM, MEM_HEADS, MEM_HEAD_DIM)),
        "state_conv": nrm(ks[9], (DEPTH, DEC_BATCH, CONV_WIDTH - 1, D_CONV)),
        "page_table": page_table,
        "g_mix": gain(ks[10], (DEPTH, D_MODEL)),
        "w_in": nrm(ks[11], (DEPTH, D_MODEL, MIX_IN), D_MODEL ** -0.5),
        "conv_w": nrm(ks[12], (DEPTH, CONV_WIDTH, D_CONV), CONV_WIDTH ** -0.5),
        "w_conv_out": nrm(ks[13], (DEPTH, D_CONV, D_MODEL), D_CONV ** -0.5),
        "w_attn_out": nrm(ks[14], (DEPTH, N_HEADS * HEAD_DIM, D_MODEL), (N_HEADS * HEAD_DIM) ** -0.5),
        "w_o": nrm(ks[15], (DEPTH, D_MODEL, D_MODEL), D_MODEL ** -0.5),
        "g_mem": gain(ks[16], (DEPTH, D_MODEL)),
        "g_mem_kv": gain(ks[17], (DEPTH, D_MODEL)),
        "w_mq": nrm(ks[18], (DEPTH, D_MODEL, D_MODEL), D_MODEL ** -0.5),
        "w_mk": nrm(ks[19], (DEPTH, D_MODEL, D_MODEL), D_MODEL ** -0.5),
        "w_mv": nrm(ks[20], (DEPTH, D_MODEL, D_MODEL), D_MODEL ** -0.5),
        "w_mo": nrm(ks[21], (DEPTH, D_MODEL, D_MODEL), D_MODEL ** -0.5),
        "g_ffn": gain(ks[22], (DEPTH, D_MODEL)),
        "w_gate": nrm(ks[23], (DEPTH, D_MODEL, D_FF), D_MODEL ** -0.5),
        "w_up": nrm(ks[24], (DEPTH, D_MODEL, D_FF), D_MODEL ** -0.5),
        "w_down": nrm(ks[25], (DEPTH, D_FF, D_MODEL), D_FF ** -0.5),
        "g_final": gain(ks[26], (D_MODEL,)),
    }


def reference(x_prompt, x_sample, mem_prompt, cache_k, cache_v, cache_idx_k, cache_mem_k, cache_mem_v,
              state_conv, page_table, g_mix, w_in, conv_w, w_conv_out, w_attn_out, w_o, g_mem, g_mem_kv,
              w_mq, w_mk, w_mv, w_mo, g_ffn, w_gate, w_up, w_down, g_final):
    xp, xs = x_prompt, x_sample
    kp_l, vp_l, ikp_l, convp_l, mkp_l, mvp_l = [], [], [], [], [], []
    ks_l, vs_l, iks_l, convs_l = [], [], [], []
    for l in range(DEPTH):
        lw = (g_mix[l], w_in[l], conv_w[l], w_conv_out[l], w_attn_out[l], w_o[l],
              g_mem[l], w_mq[l], w_mo[l], g_ffn[l], w_gate[l], w_up[l], w_down[l])
        mk_p, mv_p = memory_kv(mem_prompt, g_mem_kv[l], w_mk[l], w_mv[l])
        conv_prev_p = jnp.zeros((xp.shape[0], CONV_WIDTH - 1, D_CONV), xp.dtype)
        xp, conv_p, k_p, v_p, ik_p = trunk_layer(xp, conv_prev_p, prompt_sparse_attention, mk_p, mv_p, lw)
        attend_s = functools.partial(sample_sparse_attention, cache_k=cache_k[l], cache_v=cache_v[l],
                                     cache_ik=cache_idx_k[l], page_table=page_table)
        xs, conv_s, k_s, v_s, ik_s = trunk_layer(xs, state_conv[l], attend_s, cache_mem_k[l], cache_mem_v[l], lw)
        kp_l.append(k_p); vp_l.append(v_p); ikp_l.append(ik_p); convp_l.append(conv_p)
        mkp_l.append(mk_p); mvp_l.append(mv_p)
        ks_l.append(k_s); vs_l.append(v_s); iks_l.append(ik_s); convs_l.append(conv_s)
    y_prompt = rms_norm(xp, g_final)
    y_sample = rms_norm(xs, g_final)
    return (y_prompt, y_sample,
            jnp.stack(kp_l), jnp.stack(vp_l), jnp.stack(ikp_l), jnp.stack(convp_l),
            jnp.stack(mkp_l), jnp.stack(mvp_l),
            jnp.stack(ks_l), jnp.stack(vs_l), jnp.stack(iks_l), jnp.stack(convs_l))
```

```python
from contextlib import ExitStack
import numpy as np
import concourse.bass as bass
import concourse.mybir as mybir
from concourse.bass_utils import run_bass_kernel_spmd

F32 = mybir.dt.float32
BF16 = mybir.dt.bfloat16
I32 = mybir.dt.int32
AF = mybir.ActivationFunctionType
ALU = mybir.AluOpType
AX = mybir.AxisListType

PE, ACT, DVE, POOL, SP = "pe", "act", "dve", "pool", "sp"
COMPUTE = (PE, ACT, DVE, POOL)

D = 1024
SEQ = 8192
NT = 4
TT = 512
MIXW = 6980
C_IN, C_B, C_C, C_Q, C_K, C_V, C_IQ, C_IK, C_IW, C_GA, C_GB = 0, 1024, 2048, 3072, 4096, 4352, 4608, 4864, 4928, 4932, 5956
DFF = 2816
EPS = 1e-6
NBIS = 22
DVE_FRAC = 0.55
PIPE = True
JUNK_BC = True
NEGM = -30000.0
TOPK = 256.0
ATT_SCALE = 128 ** -0.5
MEM_SCALE = 256 ** -0.5


class _St:
    __slots__ = ("last_w", "readers")

    def __init__(self):
        self.last_w = None
        self.readers = {}


class Buf:
    def __init__(self, name, t=None, sts=None):
        self.name = name
        self.t = t
        self.sts = sts if sts is not None else [_St()]

    def alias(self, name, t=None):
        return Buf(name, t if t is not None else self.t, self.sts)


class Sched:
    def __init__(self, nc):
        self.nc = nc
        self.ops = {e: [] for e in (PE, ACT, DVE, POOL, SP)}
        self.sig = {e: 0 for e in COMPUTE}
        self.pending = {e: [] for e in COMPUTE}
        self.known = {e: {} for e in self.ops}
        self.dma_cnt = {}
        self.sems = {}

    def _waits_for(self, eng, ev, waits):
        if ev is None:
            return
        if ev[0] == "c":
            if ev[1] == eng and eng == PE:
                return
            if ev[2][0] is None:
                raise RuntimeError("dependency on unsignaled op %s (consumer %s)" % (ev[1], eng))
            key, val = ("c", ev[1]), ev[2][0]
        else:
            key = ev[1]
            val = self.dma_cnt[key]
        if self.known[eng].get(key, 0) >= val:
            return
        waits[key] = max(waits.get(key, 0), val)

    def op(self, eng, fn, reads=(), writes=(), signal=True, dma_sem=None):
        waits = {}
        for b in reads:
            for st in b.sts:
                self._waits_for(eng, st.last_w, waits)
        for b in writes:
            for st in b.sts:
                self._waits_for(eng, st.last_w, waits)
                for r in st.readers.values():
                    self._waits_for(eng, r, waits)
        for k, v in waits.items():
            self.known[eng][k] = v
        if dma_sem is not None:
            self.dma_cnt[dma_sem] = self.dma_cnt.get(dma_sem, 0) + 16
            ev = ("d", dma_sem, self.dma_cnt[dma_sem])
            inc = (dma_sem, 16)
        else:
            box = [None]
            ev = ("c", eng, box)
            self.pending[eng].append(box)
            inc = None
            if signal:
                self.sig[eng] += 1
                for bx in self.pending[eng]:
                    bx[0] = self.sig[eng]
                self.pending[eng] = []
                inc = (("c", eng), 1)
        rk = ev[1]
        for b in reads:
            for st in b.sts:
                st.readers[rk] = ev
        for b in writes:
            for st in b.sts:
                st.last_w = ev
                st.readers = {}
        self.ops[eng].append((list(waits.items()), fn, inc))
        return ev

    def dma(self, queue, out_ap, in_ap, reads=(), writes=(), sem=None, **kw):
        return self.op(queue, lambda e: e.dma_start(out=out_ap, in_=in_ap, **kw),
                       reads=reads, writes=writes, dma_sem=("d", sem))

    def finish(self, eng=SP):
        waits = [(k, v) for k, v in self.dma_cnt.items() if self.known[eng].get(k, 0) < v]
        self.ops[eng].append((waits, None, None))

    def replay(self, ctx):
        nc = self.nc
        keys = set()
        for e in self.ops:
            for waits, fn, inc in self.ops[e]:
                for k, _ in waits:
                    keys.add(k)
                if inc:
                    keys.add(inc[0])
        for k in sorted(keys, key=str):
            self.sems[k] = ctx.enter_context(nc.semaphore("s_" + "_".join(str(x) for x in k)))
        block = ctx.enter_context(nc.Block())
        engmap = {PE: block.tensor, ACT: block.scalar, DVE: block.vector, POOL: block.gpsimd, SP: block.sync}
        for e, deco in engmap.items():
            oplist = self.ops[e]
            if not oplist:
                continue

            def body(engobj, oplist=oplist):
                for waits, fn, inc in oplist:
                    for k, v in waits:
                        engobj.wait_ge(self.sems[k], v)
                    if fn is None:
                        continue
                    ins = fn(engobj)
                    if inc:
                        ins.then_inc(self.sems[inc[0]], inc[1])
            deco(body)


def build_program(with_sample=True, do_prompt=True, dbg=False):
    nc = bass.Bass("TRN2", target_bir_lowering=False)

    def din(name, shape, dt=F32):
        return nc.dram_tensor(name, list(shape), dt, kind="ExternalInput").ap()

    def dout(name, shape, dt=F32):
        return nc.dram_tensor(name, list(shape), dt, kind="ExternalOutput").ap()

    x_all = din("x_all", [SEQ, D])
    x_own = din("x_own", [NT * TT, D])
    x_halo = din("x_halo", [8, D])
    qrel_d = din("qrel", [128, 1])
    mem_d = din("mem", [256, D])
    w_in = din("w_in", [D, MIXW])
    conv_w = din("conv_w", [3, D])
    w_conv_out = din("w_conv_out", [D, D])
    w_attn_out = din("w_attn_out", [D, D])
    w_o = din("w_o", [D, D])
    w_mq = din("w_mq", [D, D])
    w_mk = din("w_mk", [D, D])
    w_mv = din("w_mv", [D, D])
    w_mo = din("w_mo", [D, D])
    w_gate = din("w_gate", [D, DFF])
    w_up = din("w_up", [D, DFF])
    w_down = din("w_down", [DFF, D])
    g_mix = din("g_mix", [D])
    g_mem = din("g_mem", [D])
    g_mem_kv = din("g_mem_kv", [D])
    g_ffn = din("g_ffn", [D])
    g_final = din("g_final", [D])

    if with_sample:
        xs_d = din("xs", [16, D])
        sconv_d = din("sconv", [16, 2, D])
        ptab_d = din("ptab", [16, 16], I32)
        cache_k = din("cache_k", [2560 * 8, 4096])
        cache_v = din("cache_v", [2560 * 8, 4096])
        cache_ik = din("cache_ik", [2560 * 8, 1024])
        cmk_d = din("cmk", [16, 256, D])
        cmv_d = din("cmv", [16, 256, D])
        o_ys = dout("o_ys", [16, D])
        o_ks = dout("o_ks", [16, 256])
        o_vs = dout("o_vs", [16, 256])
        o_iks = dout("o_iks", [16, 64])
        o_convs = dout("o_convs", [16, 2, D])

    if dbg:
        d_x1 = dout("d_x1", [16, D]); d_x2 = dout("d_x2", [16, D]); d_attn = dout("d_attn", [128, 8, 16], BF16); d_mg = dout("d_mg", [16, D])
        d_ssm = dout("d_ssm", [128, 256]); d_sel = dout("d_sel", [128, 256]); d_m16 = dout("d_m16", [128, 256]); d_om = dout("d_om", [16, D]); d_offs = dout("d_offs", [128, 16], I32); d_ikg = dout("d_ikg", [128, 1024]); d_rep = dout("d_rep", [128, 324])
    o_y = dout("o_y", [NT * TT, D])
    o_k = dout("o_k", [NT * TT, 256])
    o_v = dout("o_v", [NT * TT, 256])
    o_ik = dout("o_ik", [NT * TT, 64])
    o_conv = dout("o_conv", [2, D])
    o_mk = dout("o_mk", [256, D])
    o_mv = dout("o_mv", [256, D])

    ctx = ExitStack()
    S = Sched(nc)

    def sb(name, shape, dt):
        return Buf(name, ctx.enter_context(nc.sbuf_tensor(name, list(shape), dt)))

    KT = sb("KT", [128, 2, SEQ], BF16)
    Vb = sb("Vb", [128, 64, 256], BF16)
    ikT = sb("ikT", [128, SEQ], BF16)
    NWB = 3
    wb = [sb("wb%d" % i, [128, 8, 512], BF16) for i in range(NWB)]
    hT = sb("hT", [128, 8, TT], BF16)
    QT = sb("QT", [128, 8, TT], BF16)
    iqT = sb("iqT", [128, 2, TT], BF16)
    maT = sb("maT", [128, 8, TT], BF16)
    attnT = sb("attnT", [128, 8, TT], BF16)
    arena_t = ctx.enter_context(nc.sbuf_tensor("arena", [128, 12288], F32))
    hn = sb("hn", [128, D], BF16)
    hn2 = sb("hn2", [128, D], BF16)
    jkS = sb("jkS", [128, 2], BF16)
    PTb = [sb("PT%d" % i, [128, 512], BF16) for i in range(2)]
    ident = sb("ident", [128, 128], BF16)
    ones = sb("ones", [128, 128], BF16)
    gmixT = sb("gmixT", [128, 8], F32)
    gmemT = sb("gmemT", [128, 8], F32)
    gmkvT = sb("gmkvT", [128, 8], F32)
    gffnT = sb("gffnT", [128, 8], F32)
    cwT = sb("cwT", [128, 8, 3], F32)
    qrel = sb("qrel_sb", [128, 1], F32)
    negc = sb("negc", [128, 1664], BF16)
    ck = sb("ck", [128, NBIS], F32)
    sk = sb("sk", [128, NBIS], F32)
    small = sb("small", [128, 64], F32)
    epsb = sb("epsb", [128, 1], F32)
    iw_sb = sb("iw_sb", [128, 4, 4], F32)
    uhalo = sb("uhalo", [128, 8, 8], F32)
    memKT = sb("memKT", [128, 8, 256], BF16)
    memV = sb("memV", [128, 2, D], BF16)
    rden = sb("rden", [128, 512], F32)
    jk = sb("jk", [128, 2], BF16)
    jkA = sb("jkA", [128, 2], BF16)

    ar = arena_t
    stA, stB = _St(), _St()

    def av(name, ap, lo, hi):
        sts = ([stA] if lo < 8192 else []) + ([stB] if hi > 8192 else [])
        return Buf(name, ap, sts)

    xin = av("xin", ar[:, 0:4096].rearrange("p (b d) -> p b d", b=4), 0, 4096)
    ccT = av("ccT", ar[:, 4096:6144].rearrange("p (c n) -> p c n", c=4), 4096, 6144)
    uT = av("uT", ar[:, 6144:8200].rearrange("p (c n) -> p c n", c=4), 6144, 8200)
    cvT = av("cvT", ar[:, 8200:10248].rearrange("p (c n) -> p c n", c=4), 8200, 10248)
    ostg = av("ostg", ar[:, 10248:10824], 10248, 10824)
    Sidx = av("Sidx", ar[:, 0:8192], 0, 8192)
    maskb = av("maskb", ar[:, 8192:12288].bitcast(BF16), 8192, 12288)
    m_a = av("m_a", ar[:, 0:4096].rearrange("p (c n) -> p c n", c=8), 0, 4096)
    tmpA = av("tmpA", ar[:, 4096:4608], 4096, 4608)
    tmpB = av("tmpB", ar[:, 4608:5120], 4608, 5120)
    fT = av("fT", ar[:, 4096:9728].bitcast(BF16).rearrange("p (c n) -> p c n", c=22), 4096, 9728)
    ysb = av("ysb", ar[:, 9728:10752], 9728, 10752)
    gfin = av("gfin", ar[:, 10752:11776], 10752, 11776)
    tmpC = av("tmpC", ar[:, 11776:12288], 11776, 12288)
    xm = av("xm", ar[:, 0:2048].rearrange("p (b d) -> p b d", b=2), 0, 2048)
    xh = av("xh", ar[:, 0:1024].rearrange("p (b d) -> p b d", b=1), 0, 1024)

    banks = [Buf("pb%d" % i, ctx.enter_context(nc.psum_tensor("pb%d" % i, [128, 512], F32))) for i in range(8)]
    GEN = banks[:6]
    po = [banks[6], banks[6]]
    pden = [banks[7], banks[7]]
    rr = [0]

    def nb():
        b = GEN[rr[0] % len(GEN)]
        rr[0] += 1
        return b

    def mm(out_ap, lhsT, rhs, st, sp, R, W, sig):
        S.op(PE, lambda e: e.matmul(out_ap, lhsT=lhsT, rhs=rhs, start=st, stop=sp), reads=R, writes=W, signal=sig)

    def act(out_ap, in_ap, func, R, W, **kw):
        S.op(ACT, lambda e: e.activation(out=out_ap, in_=in_ap, func=func, **kw), reads=R, writes=W)

    def dve(fn, R, W):
        S.op(DVE, fn, reads=R, writes=W)

    wrr = [0]

    def wload(src_ap, kc, ncols):
        b = wb[wrr[0] % NWB]
        wrr[0] += 1
        S.dma(POOL, b.t[:, 0:kc, 0:ncols], src_ap.rearrange("(c p) n -> p c n", p=128), writes=[b], sem=b.name)
        return b

    S.op(POOL, lambda e: e.memset(ident.t[:], 0.0), writes=[ident])
    S.op(POOL, lambda e: e.affine_select(out=ident.t[:], in_=ident.t[:], pattern=[[-1, 128]], compare_op=ALU.not_equal,
                                         fill=1.0, base=0, channel_multiplier=1), reads=[ident], writes=[ident])
    I4ap = ident.t[:, :].unsqueeze(1).to_broadcast([128, 4, 128])
    S.op(POOL, lambda e: e.memset(ones.t[:], 1.0), writes=[ones])
    S.op(POOL, lambda e: e.memset(epsb.t[:], EPS), writes=[epsb])
    for k in range(NBIS):
        S.op(POOL, lambda e, k=k: e.memset(ck.t[:, k:k + 1], 2.0 * (1.0 + 1e-6) / 2.0 ** (k + 1)), writes=[ck])
    for g_d, g_s in ((g_mix, gmixT), (g_mem, gmemT), (g_mem_kv, gmkvT), (g_ffn, gffnT)):
        S.dma(SP, g_s.t[:], g_d.rearrange("(c p) -> p c", p=128), writes=[g_s], sem="const", allow_slow_non_contiguous=True)
    for j in range(3):
        S.dma(SP, cwT.t[:, :, j], conv_w[j, :].rearrange("(c p) -> p c", p=128), writes=[cwT], sem="const", allow_slow_non_contiguous=True)
    S.dma(SP, qrel.t[:], qrel_d, writes=[qrel], sem="const")
    S.op(POOL, lambda e: e.iota(Sidx.t[:, 0:1664], pattern=[[1, 1664]], base=0, channel_multiplier=0,
                                allow_small_or_imprecise_dtypes=True), writes=[Sidx])
    dve(lambda e: e.tensor_scalar(out=negc.t[:], in0=Sidx.t[:, 0:1664], scalar1=qrel.t[:, 0:1], scalar2=-1e30,
                                  op0=ALU.is_gt, op1=ALU.mult), [Sidx, qrel], [negc])

    ssq = small.alias("ssq", small.t[:, 0:4])
    rstd = small.alias("rstd", small.t[:, 4:8])

    def rstd_only(xt, nblk, P):
        for blk in range(nblk):
            act(jkS.t[:P, 0:1].to_broadcast([P, D]), xt.t[:P, blk, :], AF.Square, [xt], [jkS, ssq], accum_out=ssq.t[:P, blk:blk + 1])
            act(rstd.t[:P, blk:blk + 1], ssq.t[:P, blk:blk + 1], AF.Sqrt, [ssq, epsb], [rstd], scale=1.0 / D, bias=epsb.t[:P, 0:1])
            dve(lambda e, blk=blk: e.reciprocal(out=rstd.t[:P, blk:blk + 1], in_=rstd.t[:P, blk:blk + 1]), [rstd], [rstd])

    def norm_T(xt, nblk, P, gT, out_hT, col0=0):
        rstd_only(xt, nblk, P)
        for blk in range(nblk):
            hb = hn if blk % 2 == 0 else hn2
            dve(lambda e, blk=blk, hb=hb: e.tensor_scalar(out=hb.t[:P, :], in0=xt.t[:P, blk, :], scalar1=rstd.t[:P, blk:blk + 1],
                                                         scalar2=None, op0=ALU.mult), [xt, rstd], [hb])
            pb = nb()
            pv = pb.t[:, :].bitcast(BF16).rearrange("p (c n) -> p c n", c=8)
            for c in range(8):
                S.op(PE, lambda e, c=c, pv=pv, hb=hb: e.transpose(out=pv[:, c, :P], in_=hb.t[:P, c * 128:(c + 1) * 128],
                                                                  identity=ident.t[:P, :P]),
                     reads=[hb, ident], writes=[pb], signal=(c == 7))
            dve(lambda e, blk=blk, pv=pv: e.tensor_tensor(
                out=out_hT.t[:, :, col0 + blk * P: col0 + (blk + 1) * P], in0=pv[:, :, :P],
                in1=gT.t[:, :].unsqueeze(2).to_broadcast([128, 8, P]), op=ALU.mult), [pb, gT], [out_hT])

    def proj_fm(wbuf, wcol0, nchunks_out, rhsT, kc, N, consume, rcol0=0):
        for j in range(nchunks_out):
            pb = nb()
            for c in range(kc):
                mm(pb.t[:, :N], wbuf.t[:, c, wcol0 + j * 128: wcol0 + (j + 1) * 128], rhsT.t[:, c, rcol0:rcol0 + N],
                   c == 0, c == kc - 1, [wbuf, rhsT], [pb], c == kc - 1)
            consume(j, pb)

    S.dma(SP, xm.t[:, :, :], mem_d.rearrange("(b p) d -> p b d", p=128), writes=[xm], sem="xin")
    norm_T(xm, 2, 128, gmkvT, hT)
    for wd, od, is_k in ((w_mk, o_mk, True), (w_mv, o_mv, False)):
        for half in range(2):
            wbuf = wload(wd[:, half * 512:(half + 1) * 512], 8, 512)
            for blk in range(2):
                pb = nb()
                for c in range(8):
                    mm(pb.t[:, :], hT.t[:, c, blk * 128:(blk + 1) * 128], wbuf.t[:, c, :], c == 0, c == 7, [hT, wbuf], [pb], c == 7)
                act(tmpA.t[:, :], pb.t[:, :], AF.Copy, [pb], [tmpA])
                S.dma(SP, od[blk * 128:(blk + 1) * 128, half * 512:(half + 1) * 512], tmpA.t[:, :], reads=[tmpA], sem="o_small")
                if not is_k:
                    dve(lambda e, pb=pb, blk=blk, half=half: e.tensor_copy(out=memV.t[:, blk, half * 512:(half + 1) * 512], in_=pb.t[:, :]),
                        [pb], [memV])
            if is_k:
                def cons(j, pb, half=half):
                    act(memKT.t[:, half * 4 + j, :], pb.t[:, :256], AF.Copy, [pb], [memKT])
                proj_fm(wbuf, 0, 4, hT, 8, 256, cons)

    S.dma(SP, xh.t[:8, 0, :], x_halo, writes=[xh], sem="xin")
    norm_T(xh, 1, 8, gmixT, hT)
    cch = small.alias("cch", small.t[:, 16:24])
    for half in range(2):
        wcc = wload(w_in[:, C_C + half * 512: C_C + (half + 1) * 512], 8, 512)
        wci = wload(w_in[:, C_IN + half * 512: C_IN + (half + 1) * 512], 8, 512)
        for j in range(4):
            pb = nb()
            for c in range(8):
                mm(pb.t[:, :8], wcc.t[:, c, j * 128:(j + 1) * 128], hT.t[:, c, 0:8], c == 0, c == 7, [wcc, hT], [pb], c == 7)
            act(cch.t[:, :], pb.t[:, :8], AF.Copy, [pb], [cch])
            pb2 = nb()
            for c in range(8):
                mm(pb2.t[:, :8], wci.t[:, c, j * 128:(j + 1) * 128], hT.t[:, c, 0:8], c == 0, c == 7, [wci, hT], [pb2], c == 7)
            dve(lambda e, pb2=pb2, j=j, half=half: e.tensor_tensor(out=uhalo.t[:, half * 4 + j, :], in0=pb2.t[:, :8], in1=cch.t[:, :],
                                                                  op=ALU.mult), [pb2, cch], [uhalo])

    wkv = wload(w_in[:, C_K:C_K + 512], 8, 512)
    wik = wb[wrr[0] % NWB]
    wrr[0] += 1
    for hh in range(2):
        S.dma(POOL, wik.t[:, :, hh * 64:(hh + 1) * 64], w_in[:, C_IK:C_IK + 64].rearrange("(c p) n -> p c n", p=128),
              writes=[wik], sem=wik.name)
    xinB = Buf("xinB", ar[:, 4096:8192].rearrange("p (b d) -> p b d", b=4))
    S.op(DVE, lambda e: e.memset(small.t[:, 42:43], 0.0), reads=[], writes=[xinB, ccT])
    xbufs = [xin, xinB]

    def load_x(tile):
        xb = xbufs[tile % 2]
        S.dma(SP, xb.t[:, :, :], x_all[tile * TT:(tile + 1) * TT, :].rearrange("(b p) d -> p b d", p=128), writes=[xb],
              sem="xin%d" % (tile % 2))

    if do_prompt:
        load_x(0)
    for tile in range(SEQ // TT if do_prompt else 0):
        if tile + 1 < SEQ // TT:
            load_x(tile + 1)
        norm_T(xbufs[tile % 2], 4, 128, gmixT, hT)
        for g in range(2):
            pb = nb()
            for c in range(8):
                mm(pb.t[:, :], wkv.t[:, c, g * 128:(g + 1) * 128], hT.t[:, c, :], c == 0, c == 7, [wkv, hT], [pb], c == 7)
            act(KT.t[:, g, tile * TT:(tile + 1) * TT], pb.t[:, :], AF.Copy, [pb], [KT])
        pb = nb()
        for c in range(8):
            mm(pb.t[:, :], wik.t[:, c, 0:128], hT.t[:, c, :], c == 0, c == 7, [wik, hT], [pb], c == 7)
        act(ikT.t[:, tile * TT:(tile + 1) * TT], pb.t[:, :], AF.Copy, [pb], [ikT])
        for blk in range(4):
            pb = nb()
            for c in range(8):
                mm(pb.t[:, :256], hT.t[:, c, blk * 128:(blk + 1) * 128], wkv.t[:, c, 256:512], c == 0, c == 7, [hT, wkv], [pb], c == 7)
            dve(lambda e, pb=pb, tile=tile, blk=blk: e.tensor_copy(out=Vb.t[:, tile * 4 + blk, :], in_=pb.t[:, :256]), [pb], [Vb])

    S.op(DVE, lambda e: e.memset(small.t[:, 43:44], 0.0), reads=[], writes=[xinB, ccT, uT])
    Bq = small.alias("Bq", small.t[:, 8:9])
    cand = small.alias("cand", small.t[:, 9:10])
    cnt = small.alias("cnt", small.t[:, 10:11])
    btmp = small.alias("btmp", small.t[:, 11:12])
    ssA = Buf("ssA", small.t[:, 12:13])

    for t in range(NT if do_prompt else 0):
        tok0 = t * TT
        S.dma(SP, xin.t[:, :, :], x_own[tok0:tok0 + TT, :].rearrange("(b p) d -> p b d", p=128), writes=[xin], sem="xin")
        norm_T(xin, 4, 128, gmixT, hT)
        wkv = wload(w_in[:, C_K:C_K + 512], 8, 512)
        wsm = wload(w_in[:, C_IK:C_IK + 68], 8, 68)
        for blk in range(4):
            pb = nb()
            for c in range(8):
                mm(pb.t[:, :], hT.t[:, c, blk * 128:(blk + 1) * 128], wkv.t[:, c, :], c == 0, c == 7, [hT, wkv], [pb], c == 7)
            act(ostg.t[:, 0:512], pb.t[:, :], AF.Copy, [pb], [ostg])
            pb2 = nb()
            for c in range(8):
                mm(pb2.t[:, :68], hT.t[:, c, blk * 128:(blk + 1) * 128], wsm.t[:, c, 0:68], c == 0, c == 7, [hT, wsm], [pb2], c == 7)
            dve(lambda e, pb2=pb2: e.tensor_copy(out=ostg.t[:, 512:576], in_=pb2.t[:, 0:64]), [pb2], [ostg])
            dve(lambda e, pb2=pb2, blk=blk: e.tensor_copy(out=iw_sb.t[:, blk, :], in_=pb2.t[:, 64:68]), [pb2], [iw_sb])
            r0 = tok0 + blk * 128
            S.dma(SP, o_k[r0:r0 + 128, :], ostg.t[:, 0:256], reads=[ostg], sem="o_small")
            S.dma(SP, o_v[r0:r0 + 128, :], ostg.t[:, 256:512], reads=[ostg], sem="o_small")
            S.dma(SP, o_ik[r0:r0 + 128, :], ostg.t[:, 512:576], reads=[ostg], sem="o_small")
        wq = wload(w_in[:, C_IQ:C_IQ + 256], 8, 256)
        proj_fm(wq, 0, 2, hT, 8, TT, lambda j, pb: act(iqT.t[:, j, :], pb.t[:, :], AF.Copy, [pb], [iqT]))
        for half in range(2):
            wq = wload(w_in[:, C_Q + half * 512: C_Q + (half + 1) * 512], 8, 512)
            proj_fm(wq, 0, 4, hT, 8, TT,
                    lambda j, pb, half=half: act(QT.t[:, half * 4 + j, :], pb.t[:, :], AF.Copy, [pb], [QT]))
        for half in range(2):
            wcc = wload(w_in[:, C_C + half * 512: C_C + (half + 1) * 512], 8, 512)
            proj_fm(wcc, 0, 4, hT, 8, TT, lambda j, pb: act(ccT.t[:, j, :], pb.t[:, :], AF.Copy, [pb], [ccT]))
            wci = wload(w_in[:, C_IN + half * 512: C_IN + (half + 1) * 512], 8, 512)

            def cons_u(j, pb, half=half, t=t):
                dve(lambda e: e.tensor_tensor(out=uT.t[:, j, 2:514], in0=pb.t[:, :], in1=ccT.t[:, j, :], op=ALU.mult), [pb, ccT], [uT])
                dve(lambda e: e.tensor_copy(out=uT.t[:, j, 0:2], in_=uhalo.t[:, half * 4 + j, 2 * t:2 * t + 2]), [uhalo], [uT])
            proj_fm(wci, 0, 4, hT, 8, TT, cons_u)
            wcb = wload(w_in[:, C_B + half * 512: C_B + (half + 1) * 512], 8, 512)
            for j in range(4):
                ch = half * 4 + j
                dve(lambda e, j=j, ch=ch: e.tensor_scalar(out=cvT.t[:, j, :], in0=uT.t[:, j, 2:514], scalar1=cwT.t[:, ch, 2:3],
                                                          scalar2=None, op0=ALU.mult), [uT, cwT], [cvT])
                dve(lambda e, j=j, ch=ch: e.scalar_tensor_tensor(out=cvT.t[:, j, :], in0=uT.t[:, j, 1:513], scalar=cwT.t[:, ch, 1:2],
                                                                 in1=cvT.t[:, j, :], op0=ALU.mult, op1=ALU.add), [uT, cwT, cvT], [cvT])
                dve(lambda e, j=j, ch=ch: e.scalar_tensor_tensor(out=cvT.t[:, j, :], in0=uT.t[:, j, 0:512], scalar=cwT.t[:, ch, 0:1],
                                                                 in1=cvT.t[:, j, :], op0=ALU.mult, op1=ALU.add), [uT, cwT, cvT], [cvT])
            if t == NT - 1:
                for tk in range(2):
                    S.dma(SP, o_conv[tk, half * 512:(half + 1) * 512].rearrange("(c p) -> p c", p=128), uT.t[:, :, 512 + tk],
                          reads=[uT], sem="o_small", allow_slow_non_contiguous=True)

            def cons_b(j, pb, half=half):
                dve(lambda e: e.tensor_tensor(out=maT.t[:, half * 4 + j, :], in0=pb.t[:, :], in1=cvT.t[:, j, :], op=ALU.mult), [pb, cvT], [maT])
            proj_fm(wcb, 0, 4, hT, 8, TT, cons_b)

        def geom(r):
            nkb = 16 * t + 13 + r
            return nkb, nkb * 128, (16 * t + r) * 128, slice(r * 128, (r + 1) * 128)

        def idx_phase(r):
            nkb, nk, kbase, qs = geom(r)
            for k0 in range(0, nk, 512):
                w = min(512, nk - k0)
                pbs = []
                for h in range(4):
                    pb = nb()
                    pbs.append(pb)
                    hp = slice((h % 2) * 64, (h % 2) * 64 + 64)
                    mm(pb.t[:, :w], iqT.t[hp, h // 2, qs], ikT.t[hp, k0:k0 + w], True, True, [iqT, ikT], [pb], True)
                dve(lambda e, pb=pbs[0], k0=k0, w=w, r=r: e.tensor_scalar(out=Sidx.t[:, k0:k0 + w], in0=pb.t[:, :w], scalar1=0.0,
                                                                          scalar2=iw_sb.t[:, r, 0:1], op0=ALU.max, op1=ALU.mult),
                    [pbs[0], iw_sb], [Sidx])
                for h in range(1, 4):
                    act(pbs[h].t[:, :w], pbs[h].t[:, :w], AF.Relu, [pbs[h]], [pbs[h]])
                    dve(lambda e, pb=pbs[h], h=h, k0=k0, w=w, r=r: e.scalar_tensor_tensor(
                        out=Sidx.t[:, k0:k0 + w], in0=pb.t[:, :w], scalar=iw_sb.t[:, r, h:h + 1], in1=Sidx.t[:, k0:k0 + w],
                        op0=ALU.mult, op1=ALU.add), [pbs[h], iw_sb, Sidx], [Sidx])
            dve(lambda e, nk=nk: e.tensor_reduce(out=Bq.t[:, :], in_=Sidx.t[:, 0:nk], axis=AX.X, op=ALU.max,
                                                apply_absolute_value=True), [Sidx], [Bq])
            dve(lambda e, kbase=kbase, nk=nk: e.tensor_tensor(out=Sidx.t[:, kbase:nk], in0=Sidx.t[:, kbase:nk], in1=negc.t[:, 0:nk - kbase],
                                                             op=ALU.add), [Sidx, negc], [Sidx])
            dve(lambda e: e.tensor_scalar(out=sk.t[:, :], in0=ck.t[:, :], scalar1=Bq.t[:, 0:1], scalar2=None, op0=ALU.mult), [ck, Bq], [sk])
            dve(lambda e: e.scalar_tensor_tensor(out=cand.t[:, :], in0=Bq.t[:, :], scalar=-1.0, in1=sk.t[:, 0:1], op0=ALU.mult, op1=ALU.add),
                [Bq, sk], [cand])

        def bis_iter(r, k):
            nkb, nk, kbase, qs = geom(r)
            kd = max(1, int(round(nkb * DVE_FRAC))) * 128
            na = nk - kd
            dve(lambda e, kd=kd: e.tensor_scalar(out=(jk.t[:, 0:1].to_broadcast([128, kd]) if JUNK_BC else maskb.t[:, 0:kd]), in0=Sidx.t[:, 0:kd], scalar1=cand.t[:, 0:1], scalar2=None,
                                                op0=ALU.is_ge, op1=ALU.add, accum_out=cnt.t[:, 0:1]), [Sidx, cand], [jk if JUNK_BC else maskb, cnt])
            if na > 0:
                act(jkA.t[:, 0:1].to_broadcast([128, na]), Sidx.t[:, kd:nk], AF.Sign, [Sidx, cand], [jkA, ssA], scale=-1.0, bias=cand.t[:, 0:1],
                    accum_out=ssA.t[:, 0:1])
                dve(lambda e: e.scalar_tensor_tensor(out=cnt.t[:, :], in0=ssA.t[:, :], scalar=-0.5, in1=cnt.t[:, :], op0=ALU.mult, op1=ALU.add),
                    [ssA, cnt], [cnt])
            last = (k == NBIS - 1)
            dve(lambda e, last=last, na=na: e.tensor_scalar(out=btmp.t[:, :], in0=cnt.t[:, :], scalar1=TOPK - 0.5 * na, scalar2=(1.0 if last else 0.5),
                                                           op0=ALU.is_ge, op1=ALU.subtract), [cnt], [btmp])
            dve(lambda e, k=k: e.scalar_tensor_tensor(out=cand.t[:, :], in0=btmp.t[:, :], scalar=sk.t[:, k:k + 1], in1=cand.t[:, :],
                                                     op0=ALU.mult, op1=ALU.add), [btmp, sk, cand], [cand])

        def mask_phase(r):
            nkb, nk, kbase, qs = geom(r)
            dve(lambda e, nk=nk: e.tensor_scalar(out=maskb.t[:, 0:nk], in0=Sidx.t[:, 0:nk], scalar1=cand.t[:, 0:1], scalar2=NEGM,
                                                op0=ALU.is_lt, op1=ALU.mult), [Sidx, cand], [maskb])

        def attn_gen(r):
            nkb, nk, kbase, qs = geom(r)
            for g in range(2):
                for kb in range(nkb):
                    pb = nb()
                    mm(pb.t[:, :], KT.t[:, g, kb * 128:(kb + 1) * 128], QT.t[:, 4 * g:4 * g + 4, qs], True, False, [KT, QT], [pb], False)
                    mm(pb.t[:, :], maskb.t[:, kb * 128:(kb + 1) * 128], I4ap, False, True, [maskb, ident], [pb], True)
                    pt = PTb[kb % 2]
                    act(pt.t[:, :], pb.t[:, :], AF.Exp, [pb], [pt], scale=ATT_SCALE)
                    mm(po[g].t[:, :], Vb.t[:, kb, g * 128:(g + 1) * 128], pt.t[:, :], kb == 0, kb == nkb - 1, [Vb, pt], [po[g]], kb == nkb - 1)
                    mm(pden[g].t[:, :], ones.t[:, :], pt.t[:, :], kb == 0, kb == nkb - 1, [ones, pt], [pden[g]], kb == nkb - 1)
                    yield
                dve(lambda e, g=g: e.reciprocal(out=rden.t[:, :], in_=pden[g].t[:, :]), [pden[g]], [rden])
                dve(lambda e, g=g, qs=qs: e.tensor_tensor(out=attnT.t[:, 4 * g:4 * g + 4, qs], in0=po[g].t[:, :].rearrange("p (h q) -> p h q", h=4),
                                                         in1=rden.t[:, :].rearrange("p (h q) -> p h q", h=4), op=ALU.mult),
                    [po[g], rden], [attnT])

        if not PIPE:
            for r in range(4):
                idx_phase(r)
                for k in range(NBIS):
                    bis_iter(r, k)
                mask_phase(r)
                for _ in attn_gen(r):
                    pass
        else:
          idx_phase(0)
          for k in range(NBIS):
            bis_iter(0, k)
          mask_phase(0)
        for r in range(4 if PIPE else 0):
            gen = attn_gen(r)
            if r + 1 < 4:
                idx_phase(r + 1)
                nunits = 2 * geom(r)[0]
                per = -(-nunits // NBIS)
                alive = True
                for k in range(NBIS):
                    for _ in range(per):
                        if alive and next(gen, "done") == "done":
                            alive = False
                    bis_iter(r + 1, k)
                for _ in gen:
                    pass
                mask_phase(r + 1)
            else:
                for _ in gen:
                    pass

        mgT = QT
        for half in range(2):
            wga = wload(w_in[:, C_GA + half * 512: C_GA + (half + 1) * 512], 8, 512)
            wco = wload(w_conv_out[:, half * 512:(half + 1) * 512], 8, 512)
            for j in range(4):
                ch = half * 4 + j
                js = slice(j * 128, (j + 1) * 128)
                pga, pa = nb(), nb()
                for c in range(8):
                    mm(pga.t[:, :], wga.t[:, c, js], hT.t[:, c, :], c == 0, c == 7, [wga, hT], [pga], c == 7)
                for c in range(8):
                    mm(pa.t[:, :], wco.t[:, c, js], maT.t[:, c, :], c == 0, c == 7, [wco, maT], [pa], c == 7)
                act(tmpA.t[:, :], pga.t[:, :], AF.Sigmoid, [pga], [tmpA])
                dve(lambda e, pa=pa, ch=ch: e.tensor_tensor(out=m_a.t[:, ch, :], in0=pa.t[:, :], in1=tmpA.t[:, :], op=ALU.mult), [pa, tmpA], [m_a])
        for half in range(2):
            wgb = wload(w_in[:, C_GB + half * 512: C_GB + (half + 1) * 512], 8, 512)
            wao = wload(w_attn_out[:, half * 512:(half + 1) * 512], 8, 512)
            for j in range(4):
                ch = half * 4 + j
                js = slice(j * 128, (j + 1) * 128)
                pgb, pbb = nb(), nb()
                for c in range(8):
                    mm(pgb.t[:, :], wgb.t[:, c, js], hT.t[:, c, :], c == 0, c == 7, [wgb, hT], [pgb], c == 7)
                for c in range(8):
                    mm(pbb.t[:, :], wao.t[:, c, js], attnT.t[:, c, :], c == 0, c == 7, [wao, attnT], [pbb], c == 7)
                act(tmpB.t[:, :], pgb.t[:, :], AF.Sigmoid, [pgb], [tmpB])
                dve(lambda e, pbb=pbb: e.tensor_tensor(out=tmpB.t[:, :], in0=pbb.t[:, :], in1=tmpB.t[:, :], op=ALU.mult), [pbb, tmpB], [tmpB])
                dve(lambda e, ch=ch: e.tensor_tensor(out=mgT.t[:, ch, :], in0=m_a.t[:, ch, :], in1=tmpB.t[:, :], op=ALU.add), [m_a, tmpB], [mgT])
        S.dma(SP, xin.t[:, :, :], x_own[tok0:tok0 + TT, :].rearrange("(b p) d -> p b d", p=128), writes=[xin], sem="xin")

        def resid_add(actT, kc_total, wsrc):
            for half in range(2):
                pbs = [nb() for _ in range(4)]
                kdone = 0
                while kdone < kc_total:
                    kc = min(8, kc_total - kdone)
                    wbuf = wload(wsrc[kdone * 128:(kdone + kc) * 128, half * 512:(half + 1) * 512], kc, 512)
                    for blk in range(4):
                        for c in range(kc):
                            first = (kdone + c == 0)
                            lastc = (kdone + c == kc_total - 1)
                            mm(pbs[blk].t[:, :], actT.t[:, kdone + c, blk * 128:(blk + 1) * 128], wbuf.t[:, c, :], first, lastc,
                               [actT, wbuf], [pbs[blk]], lastc)
                    kdone += kc
                for blk in range(4):
                    dve(lambda e, blk=blk, half=half, pb=pbs[blk]: e.tensor_tensor(
                        out=xin.t[:, blk, half * 512:(half + 1) * 512], in0=pb.t[:, :], in1=xin.t[:, blk, half * 512:(half + 1) * 512],
                        op=ALU.add), [pbs[blk], xin], [xin])

        resid_add(mgT, 8, w_o)
        norm_T(xin, 4, 128, gmemT, hT)
        qmT = QT
        for half in range(2):
            wq = wload(w_mq[:, half * 512:(half + 1) * 512], 8, 512)
            proj_fm(wq, 0, 4, hT, 8, TT, lambda j, pb, half=half: act(qmT.t[:, half * 4 + j, :], pb.t[:, :], AF.Copy, [pb], [qmT]))
        omT = attnT
        for h in range(4):
            pts = []
            for kblk in range(2):
                pb = nb()
                for dd in range(2):
                    mm(pb.t[:, :], memKT.t[:, 2 * h + dd, kblk * 128:(kblk + 1) * 128], qmT.t[:, 2 * h + dd, :], dd == 0, dd == 1,
                       [memKT, qmT], [pb], dd == 1)
                pt = PTb[kblk]
                act(pt.t[:, :], pb.t[:, :], AF.Exp, [pb], [pt], scale=MEM_SCALE)
                pts.append(pt)
            for kblk in range(2):
                mm(pden[0].t[:, :], ones.t[:, :], pts[kblk].t[:, :], kblk == 0, kblk == 1, [ones, pts[kblk]], [pden[0]], kblk == 1)
            dve(lambda e: e.reciprocal(out=rden.t[:, :], in_=pden[0].t[:, :]), [pden[0]], [rden])
            for dd in range(2):
                pb = nb()
                for kblk in range(2):
                    mm(pb.t[:, :], memV.t[:, kblk, (2 * h + dd) * 128:(2 * h + dd + 1) * 128], pts[kblk].t[:, :], kblk == 0, kblk == 1,
                       [memV, pts[kblk]], [pb], kblk == 1)
                dve(lambda e, pb=pb, h=h, dd=dd: e.tensor_tensor(out=omT.t[:, 2 * h + dd, :], in0=pb.t[:, :], in1=rden.t[:, :], op=ALU.mult),
                    [pb, rden], [omT])
        resid_add(omT, 8, w_mo)
        norm_T(xin, 4, 128, gffnT, hT)
        for p0 in range(0, DFF, 512):
            ncol = min(512, DFF - p0)
            wg = wload(w_gate[:, p0:p0 + ncol], 8, ncol)
            wu = wload(w_up[:, p0:p0 + ncol], 8, ncol)
            for j in range(ncol // 128):
                fc = p0 // 128 + j
                js = slice(j * 128, (j + 1) * 128)
                pg, pu = nb(), nb()
                for c in range(8):
                    mm(pg.t[:, :], wg.t[:, c, js], hT.t[:, c, :], c == 0, c == 7, [wg, hT], [pg], c == 7)
                for c in range(8):
                    mm(pu.t[:, :], wu.t[:, c, js], hT.t[:, c, :], c == 0, c == 7, [wu, hT], [pu], c == 7)
                act(tmpC.t[:, :], pg.t[:, :], AF.Silu, [pg], [tmpC])
                dve(lambda e, pu=pu, fc=fc: e.tensor_tensor(out=fT.t[:, fc, :], in0=pu.t[:, :], in1=tmpC.t[:, :], op=ALU.mult), [pu, tmpC], [fT])
        resid_add(fT, 22, w_down)
        S.dma(SP, gfin.t[:, :], g_final.partition_broadcast(128), writes=[gfin], sem="const")
        rstd_only(xin, 4, 128)
        for blk in range(4):
            dve(lambda e, blk=blk: e.scalar_tensor_tensor(out=ysb.t[:, :], in0=xin.t[:, blk, :], scalar=rstd.t[:, blk:blk + 1], in1=gfin.t[:, :],
                                                         op0=ALU.mult, op1=ALU.mult), [xin, rstd, gfin], [ysb])
            r0 = tok0 + blk * 128
            S.dma(SP, o_y[r0:r0 + 128, :], ysb.t[:, :], reads=[ysb], sem="o_y")


    if not with_sample:
        S.finish(SP)
        S.replay(ctx)
        ctx.close()
        return nc
    NS = 16
    NBS = 30
    R1 = KT.t[:, :, :].rearrange("p g k -> p (g k)").bitcast(F32)
    R2 = Vb.t[:, :, :].rearrange("p k c -> p (k c)").bitcast(F32)
    R3 = ikT.t[:, :].bitcast(F32)
    views = []

    def sv(name, ap):
        b = Buf(name, ap)
        views.append(b)
        return b

    Kg = sv("Kg", R1[:, 0:4096])
    Vg = sv("Vg", R1[:, 4096:8192])
    KTs = sv("KTs", R2[:, 0:2048].bitcast(BF16).rearrange("p (j k) -> p j k", j=32))
    ikg = sv("ikg", R2[:, 2048:3072])
    tmpi = sv("tmpi", R2[:, 3072:4096])
    q_tm = sv("q_tm", R2[:, 4096:5120])
    kv_tm = sv("kv_tm", R2[:, 5120:5632])
    smt = sv("smt", R2[:, 5632:5956])
    xs_sb = sv("xs_sb", R2[:, 6144:7168].rearrange("p (b d) -> p b d", b=1))
    omtm = sv("omtm", R2[:, 7168:8192])
    Ssm = sv("Ssm", R3[:, 0:256].rearrange("p (b s) -> p b s", b=16))
    sel = sv("sel", R3[:, 256:512].rearrange("p (b s) -> p b s", b=16))
    cmpb = sv("cmpb", R3[:, 512:640].bitcast(BF16).rearrange("p (b s) -> p b s", b=16))
    m16 = [sv("m16_%d" % i, R3[:, 640 + 16 * i: 656 + 16 * i]) for i in range(16)]
    cntb = sv("cntb", R3[:, 896:912])
    PTs = sv("PTs", R3[:, 1024:1152].rearrange("p (s k) -> p s k", s=16))
    rep_sb = sv("rep_sb", R3[:, 1152:1476])
    o_sb = sv("o_sb", R3[:, 1536:1792])
    Pr = sv("Pr", R3[:, 1792:1800])
    Pn = sv("Pn", R3[:, 1800:1928].rearrange("p (b k) -> p b k", b=16))
    pnew = sv("pnew", R3[:, 1928:1936])
    s16 = [sv("s16_%d" % i, R3[:, 1936 + 8 * i: 1944 + 8 * i]) for i in range(8)]
    offs_f = sv("offs_f", R3[:, 2048:2064])
    offs_i = sv("offs_i", R3[:, 2064:2080].bitcast(I32))
    ptT = sv("ptT", R3[:, 2080:2096].bitcast(I32))
    ptTf = sv("ptTf", R3[:, 2096:2112])
    pm8 = sv("pm8", R3[:, 2112:2113])
    idf = sv("idf", R3[:, 2176:2304])
    onesf = sv("onesf", R3[:, 2304:2432])
    Amat = sv("Amat", R3[:, 2432:2560])
    tmpq = sv("tmpq", R3[:, 2560:3584])
    cmk_b = Kg.alias("cmk_b", R1[:, 0:2048].rearrange("p (m d) -> p m d", m=2))
    cmv_b = Kg.alias("cmv_b", R1[:, 2048:4096].rearrange("p (m d) -> p m d", m=2))
    tmpm = Vg.alias("tmpm", R1[:, 4096:6144].rearrange("p (m d) -> p m d", m=2))
    S.op(DVE, lambda e: e.memset(small.t[:, 40:41], 0.0), reads=[], writes=[KT, Vb, ikT] + views)

    zci = av("zci", ar[:16, 0:1024], 0, 1024)
    zcc = av("zcc", ar[:16, 1024:2048], 1024, 2048)
    zcb = av("zcb", ar[:16, 2048:3072], 2048, 3072)
    prev = av("prev", ar[:16, 3072:5120].rearrange("p (j d) -> p j d", j=2), 3072, 5120)
    cwb = av("cwb", ar[:16, 5120:8192].rearrange("p (j d) -> p j d", j=3), 5120, 8192)
    mt1 = av("mt1", ar[:16, 8192:9216], 8192, 9216)
    mt2 = av("mt2", ar[:16, 9216:10240], 9216, 10240)
    g_tm = av("g_tm", ar[:16, 0:2816], 0, 2816)
    u_tm = av("u_tm", ar[:16, 2816:5632], 2816, 5632)

    def proj_tm(actT, kc_total, wsrc, col_lo, ncols_total, consume):
        for c0 in range(0, ncols_total, 512):
            ncol = min(512, ncols_total - c0)
            pb = nb()
            kdone = 0
            while kdone < kc_total:
                kc = min(8, kc_total - kdone)
                wbuf = wload(wsrc[kdone * 128:(kdone + kc) * 128, col_lo + c0: col_lo + c0 + ncol], kc, ncol)
                for c in range(kc):
                    first = (kdone + c == 0)
                    lastc = (kdone + c == kc_total - 1)
                    mm(pb.t[:NS, :ncol], actT.t[:, kdone + c, 0:NS], wbuf.t[:, c, :ncol], first, lastc, [actT, wbuf], [pb], lastc)
                kdone += kc
            consume(c0, ncol, pb)

    def tm_to_fm(src, nchunks, dst):
        for c0 in range(0, nchunks, 8):
            n = min(8, nchunks - c0)
            dve(lambda e, c0=c0, n=n: e.tensor_copy(out=hn.t[:NS, 0:n * 128], in_=src.t[:NS, c0 * 128:(c0 + n) * 128]), [src], [hn])
            pb = nb()
            pv = pb.t[:, :].bitcast(BF16).rearrange("p (c n) -> p c n", c=8)
            for c in range(n):
                S.op(PE, lambda e, c=c, pv=pv: e.transpose(out=pv[:, c, :NS], in_=hn.t[:NS, c * 128:(c + 1) * 128], identity=ident.t[:NS, :NS]),
                     reads=[hn, ident], writes=[pb], signal=(c == n - 1))
            act(dst.t[:, c0:c0 + n, 0:NS], pv[:, 0:n, :NS], AF.Copy, [pb], [dst])

    def copy_out(dstv, col0):
        return lambda c0, ncol, pb: act(dstv.t[:NS, col0 + c0: col0 + c0 + ncol], pb.t[:NS, :ncol], AF.Copy, [pb], [dstv])

    S.op(POOL, lambda e: e.memset(idf.t[:, :], 0.0), writes=[idf])
    S.op(POOL, lambda e: e.affine_select(out=idf.t[:, :], in_=idf.t[:, :], pattern=[[-1, 128]], compare_op=ALU.not_equal,
                                         fill=1.0, base=0, channel_multiplier=1), reads=[idf], writes=[idf])
    S.op(POOL, lambda e: e.memset(onesf.t[:, :], 1.0), writes=[onesf])
    S.op(POOL, lambda e: e.memset(Amat.t[:, :], 1.0), writes=[Amat])
    S.op(POOL, lambda e: e.affine_select(out=Amat.t[:NS, :], in_=Amat.t[:NS, :], pattern=[[1, 128]], compare_op=ALU.is_ge,
                                         fill=0.0, base=0, channel_multiplier=-8), reads=[Amat], writes=[Amat])
    S.op(POOL, lambda e: e.affine_select(out=Amat.t[:NS, :], in_=Amat.t[:NS, :], pattern=[[-1, 128]], compare_op=ALU.is_ge,
                                         fill=0.0, base=7, channel_multiplier=8), reads=[Amat], writes=[Amat])
    S.op(POOL, lambda e: e.iota(pm8.t[:, :], pattern=[[0, 1]], base=0, channel_multiplier=1, allow_small_or_imprecise_dtypes=True), writes=[pm8])
    pbo = nb()
    mm(pbo.t[:, 16:17], Amat.t[:NS, :], pm8.t[:NS, :], True, True, [Amat, pm8], [pbo], True)
    dve(lambda e, pbo=pbo: e.scalar_tensor_tensor(out=pm8.t[:, :], in0=pbo.t[:, 16:17], scalar=-8.0, in1=pm8.t[:, :], op0=ALU.mult, op1=ALU.add),
        [pbo, pm8], [pm8])
    S.dma(SP, ptT.t[:NS, :], ptab_d.rearrange("b n -> n b"), writes=[ptT], sem="const", allow_slow_non_contiguous=True)
    dve(lambda e: e.tensor_copy(out=ptTf.t[:NS, :], in_=ptT.t[:NS, :]), [ptT], [ptTf])
    pbo = nb()
    mm(pbo.t[:, :NS], Amat.t[:NS, :], ptTf.t[:NS, :], True, True, [Amat, ptTf], [pbo], True)
    dve(lambda e, pbo=pbo: e.tensor_scalar(out=offs_f.t[:, :], in0=pbo.t[:, :NS], scalar1=8.0, scalar2=pm8.t[:, 0:1], op0=ALU.mult, op1=ALU.add),
        [pbo, pm8], [offs_f])
    dve(lambda e: e.tensor_copy(out=offs_i.t[:, :], in_=offs_f.t[:, :]), [offs_f], [offs_i])

    S.dma(SP, xs_sb.t[:NS, 0, :], xs_d, writes=[xs_sb], sem="xin")
    norm_T(xs_sb, 1, NS, gmixT, hT)
    proj_tm(hT, 8, w_in, C_K, 512, copy_out(kv_tm, 0))
    proj_tm(hT, 8, w_in, C_IQ, 324, copy_out(smt, 0))
    proj_tm(hT, 8, w_in, C_Q, 1024, copy_out(q_tm, 0))
    S.dma(SP, o_ks, kv_tm.t[:NS, 0:256], reads=[kv_tm], sem="o_small")
    S.dma(SP, o_vs, kv_tm.t[:NS, 256:512], reads=[kv_tm], sem="o_small")
    S.dma(SP, o_iks, smt.t[:NS, 256:320], reads=[smt], sem="o_small")
    tm_to_fm(q_tm, 8, QT)
    proj_tm(hT, 8, w_in, C_IN, 1024, copy_out(zci, 0))
    proj_tm(hT, 8, w_in, C_C, 1024, copy_out(zcc, 0))
    proj_tm(hT, 8, w_in, C_B, 1024, copy_out(zcb, 0))
    S.dma(SP, prev.t[:, :, :], sconv_d, writes=[prev], sem="xin")
    for j in range(3):
        S.dma(SP, cwb.t[:, j, :], conv_w[j, :].partition_broadcast(NS), writes=[cwb], sem="xin")
    dve(lambda e: e.tensor_tensor(out=zci.t[:, :], in0=zci.t[:, :], in1=zcc.t[:, :], op=ALU.mult), [zci, zcc], [zci])
    S.dma(SP, o_convs[:, 0, :], prev.t[:, 1, :], reads=[prev], sem="o_small")
    S.dma(SP, o_convs[:, 1, :], zci.t[:, :], reads=[zci], sem="o_small")
    dve(lambda e: e.tensor_tensor(out=zcc.t[:, :], in0=zci.t[:, :], in1=cwb.t[:, 2, :], op=ALU.mult), [zci, cwb], [zcc])
    dve(lambda e: e.tensor_tensor(out=mt1.t[:, :], in0=prev.t[:, 1, :], in1=cwb.t[:, 1, :], op=ALU.mult), [prev, cwb], [mt1])
    dve(lambda e: e.tensor_tensor(out=zcc.t[:, :], in0=zcc.t[:, :], in1=mt1.t[:, :], op=ALU.add), [zcc, mt1], [zcc])
    dve(lambda e: e.tensor_tensor(out=mt1.t[:, :], in0=prev.t[:, 0, :], in1=cwb.t[:, 0, :], op=ALU.mult), [prev, cwb], [mt1])
    dve(lambda e: e.tensor_tensor(out=zcc.t[:, :], in0=zcc.t[:, :], in1=mt1.t[:, :], op=ALU.add), [zcc, mt1], [zcc])
    dve(lambda e: e.tensor_tensor(out=zcb.t[:, :], in0=zcb.t[:, :], in1=zcc.t[:, :], op=ALU.mult), [zcb, zcc], [zcb])
    tm_to_fm(zcb, 8, maT)
    proj_tm(maT, 8, w_conv_out, 0, 1024, copy_out(mt1, 0))
    proj_tm(hT, 8, w_in, C_GA, 1024, lambda c0, ncol, pb: act(mt2.t[:, c0:c0 + ncol], pb.t[:NS, :ncol], AF.Sigmoid, [pb], [mt2]))
    dve(lambda e: e.tensor_tensor(out=mt1.t[:, :], in0=mt1.t[:, :], in1=mt2.t[:, :], op=ALU.mult), [mt1, mt2], [mt1])

    snew, snew_rep, Bs, candS, totS, bt1, bt2, tauS, am, selnr = m16[0], m16[1], m16[2], m16[3], m16[4], m16[5], m16[6], m16[7], m16[8], m16[9]
    ikg2 = av("ikg2", ar[:, 11264:12288], 11264, 12288)
    ikgB = [ikg, ikg2]

    def gather_ik(b):
        dst = ikgB[b % 2]
        S.op(POOL, lambda e, b=b, dst=dst: e.indirect_dma_start(out=dst.t[:, :], out_offset=None, in_=cache_ik[:, :],
                                                                in_offset=bass.IndirectOffsetOnAxis(ap=offs_i.t[:, b:b + 1], axis=0)),
             reads=[offs_i], writes=[dst], dma_sem=("d", "gik%d" % (b % 2)))

    gather_ik(0)
    for b in range(NS):
        if b + 1 < NS:
            gather_ik(b + 1)
        ikg = ikgB[b % 2]
        pb = nb()
        mm(pb.t[:, :324], idf.t[0:NS, b:b + 1].to_broadcast([NS, 128]), smt.t[:NS, :], True, True, [idf, smt], [pb], True)
        act(rep_sb.t[:, :], pb.t[:, :324], AF.Copy, [pb], [rep_sb])
        for h in range(4):
            dve(lambda e, h=h, ikg=ikg: e.tensor_tensor(out=tmpi.t[:, :].rearrange("p (s d) -> p s d", s=16),
                                               in0=ikg.t[:, :].rearrange("p (s d) -> p s d", s=16),
                                               in1=rep_sb.t[:, h * 64:(h + 1) * 64].unsqueeze(1).to_broadcast([128, 16, 64]), op=ALU.mult),
                [ikg, rep_sb], [tmpi])
            dve(lambda e: e.tensor_reduce(out=bt1.t[:, :], in_=tmpi.t[:, :].rearrange("p (s d) -> p s d", s=16), axis=AX.X, op=ALU.add),
                [tmpi], [bt1])
            if h == 0:
                dve(lambda e, b=b: e.tensor_scalar(out=Ssm.t[:, b, :], in0=bt1.t[:, :], scalar1=0.0, scalar2=rep_sb.t[:, 320:321],
                                                   op0=ALU.max, op1=ALU.mult), [bt1, rep_sb], [Ssm])
            else:
                dve(lambda e, h=h: e.tensor_scalar(out=bt1.t[:, :], in0=bt1.t[:, :], scalar1=0.0, scalar2=rep_sb.t[:, 320 + h:321 + h],
                                                   op0=ALU.max, op1=ALU.mult), [bt1, rep_sb], [bt1])
                dve(lambda e, b=b: e.tensor_tensor(out=Ssm.t[:, b, :], in0=Ssm.t[:, b, :], in1=bt1.t[:, :], op=ALU.add), [Ssm, bt1], [Ssm])
    if dbg:
        S.dma(SP, d_offs, offs_i.t[:, :], reads=[offs_i], sem="dbg")
        S.dma(SP, d_ikg, ikg.t[:, :], reads=[ikg], sem="dbg")
        S.dma(SP, d_rep, rep_sb.t[:, :], reads=[rep_sb], sem="dbg")
    dve(lambda e: e.tensor_tensor(out=tmpq.t[:NS, 0:256].rearrange("p (h d) -> p h d", h=4), in0=smt.t[:NS, 0:256].rearrange("p (h d) -> p h d", h=4),
                                  in1=smt.t[:NS, 256:320].unsqueeze(1).to_broadcast([NS, 4, 64]), op=ALU.mult), [smt], [tmpq])
    dve(lambda e: e.tensor_reduce(out=s16[0].t[:NS, 0:4], in_=tmpq.t[:NS, 0:256].rearrange("p (h d) -> p h d", h=4), axis=AX.X, op=ALU.add),
        [tmpq], [s16[0]])
    dve(lambda e: e.tensor_scalar(out=s16[0].t[:NS, 0:4], in0=s16[0].t[:NS, 0:4], scalar1=0.0, scalar2=None, op0=ALU.max), [s16[0]], [s16[0]])
    dve(lambda e: e.tensor_tensor(out=s16[0].t[:NS, 0:4], in0=s16[0].t[:NS, 0:4], in1=smt.t[:NS, 320:324], op=ALU.mult), [s16[0], smt], [s16[0]])
    dve(lambda e: e.tensor_reduce(out=snew.t[:NS, 0:1], in_=s16[0].t[:NS, 0:4], axis=AX.X, op=ALU.add), [s16[0]], [snew])

    def rep_diag(src_col, dst):
        dve(lambda e: e.tensor_scalar(out=bt2.t[:NS, :], in0=idf.t[:NS, 0:NS], scalar1=src_col, scalar2=None, op0=ALU.mult), [idf, snew, s16[1]], [bt2])
        pbx = nb()
        mm(pbx.t[:, :NS], onesf.t[:NS, :], bt2.t[:NS, :], True, True, [onesf, bt2], [pbx], True)
        dve(lambda e: e.tensor_copy(out=dst.t[:, :], in_=pbx.t[:, :NS]), [pbx], [dst])

    rep_diag(snew.t[:NS, 0:1], snew_rep)
    dve(lambda e: e.tensor_reduce(out=am.t[:, :], in_=Ssm.t[:, :, :], axis=AX.X, op=ALU.max, apply_absolute_value=True), [Ssm], [am])
    dve(lambda e: e.tensor_reduce(out=bt1.t[:, :], in_=snew_rep.t[:, :].unsqueeze(2), axis=AX.X, op=ALU.max, apply_absolute_value=True), [snew_rep], [bt1])
    dve(lambda e: e.tensor_tensor(out=am.t[:, :], in0=am.t[:, :], in1=bt1.t[:, :], op=ALU.add), [am, bt1], [am])
    pbx = nb()
    mm(pbx.t[:, :NS], onesf.t[:, :], am.t[:, :], True, True, [onesf, am], [pbx], True)
    dve(lambda e, pbx=pbx: e.tensor_copy(out=Bs.t[:, :], in_=pbx.t[:, :NS]), [pbx], [Bs])
    c0k = 2.0 * (1.0 + 1e-6) / 2.0
    dve(lambda e: e.tensor_scalar(out=candS.t[:, :], in0=Bs.t[:, :], scalar1=c0k - 1.0, scalar2=None, op0=ALU.mult), [Bs], [candS])
    for k in range(NBS):
        ckk = 2.0 * (1.0 + 1e-6) / 2.0 ** (k + 1)
        dve(lambda e: e.tensor_tensor(out=cmpb.t[:, :, :], in0=Ssm.t[:, :, :], in1=candS.t[:, :].unsqueeze(2).to_broadcast([128, 16, 16]),
                                      op=ALU.is_ge), [Ssm, candS], [cmpb])
        dve(lambda e: e.tensor_reduce(out=cntb.t[:, :], in_=cmpb.t[:, :, :], axis=AX.X, op=ALU.add), [cmpb], [cntb])
        pbx = nb()
        mm(pbx.t[:, :NS], onesf.t[:, :], cntb.t[:, :], True, True, [onesf, cntb], [pbx], True)
        dve(lambda e: e.tensor_tensor(out=bt1.t[:, :], in0=snew_rep.t[:, :], in1=candS.t[:, :], op=ALU.is_ge), [snew_rep, candS], [bt1])
        dve(lambda e, pbx=pbx: e.tensor_tensor(out=totS.t[:, :], in0=pbx.t[:, :NS], in1=bt1.t[:, :], op=ALU.add), [pbx, bt1], [totS])
        last = (k == NBS - 1)
        dve(lambda e, last=last: e.tensor_scalar(out=bt1.t[:, :], in0=totS.t[:, :], scalar1=TOPK, scalar2=(1.0 if last else 0.5),
                                                op0=ALU.is_ge, op1=ALU.subtract), [totS], [bt1])
        dve(lambda e: e.tensor_tensor(out=bt1.t[:, :], in0=bt1.t[:, :], in1=Bs.t[:, :], op=ALU.mult), [bt1, Bs], [bt1])
        dve(lambda e, ckk=ckk: e.scalar_tensor_tensor(out=candS.t[:, :], in0=bt1.t[:, :], scalar=ckk, in1=candS.t[:, :], op0=ALU.mult, op1=ALU.add),
            [bt1, candS], [candS])
    dve(lambda e: e.tensor_tensor(out=sel.t[:, :, :], in0=Ssm.t[:, :, :], in1=candS.t[:, :].unsqueeze(2).to_broadcast([128, 16, 16]), op=ALU.is_ge),
        [Ssm, candS], [sel])
    dve(lambda e: e.tensor_tensor(out=selnr.t[:, :], in0=snew_rep.t[:, :], in1=candS.t[:, :], op=ALU.is_ge), [snew_rep, candS], [selnr])
    dve(lambda e: e.tensor_tensor(out=bt2.t[:NS, :], in0=selnr.t[:NS, :], in1=idf.t[:NS, 0:NS], op=ALU.mult), [selnr, idf], [bt2])
    seln = s16[2]
    dve(lambda e: e.tensor_reduce(out=seln.t[:NS, 0:1], in_=bt2.t[:NS, :], axis=AX.X, op=ALU.add), [bt2], [seln])
    dve(lambda e: e.tensor_tensor(out=tmpq.t[:NS, :].rearrange("p (g h d) -> p g h d", g=2, h=4),
                                  in0=q_tm.t[:NS, :].rearrange("p (g h d) -> p g h d", g=2, h=4),
                                  in1=kv_tm.t[:NS, 0:256].rearrange("p (g d) -> p g d", g=2).unsqueeze(2).to_broadcast([NS, 2, 4, 128]),
                                  op=ALU.mult), [q_tm, kv_tm], [tmpq])
    dve(lambda e: e.tensor_reduce(out=pnew.t[:NS, :], in_=tmpq.t[:NS, :].rearrange("p (k d) -> p k d", k=8), axis=AX.X, op=ALU.add), [tmpq], [pnew])
    act(pnew.t[:NS, :], pnew.t[:NS, :], AF.Exp, [pnew], [pnew], scale=ATT_SCALE)
    dve(lambda e: e.tensor_scalar(out=pnew.t[:NS, :], in0=pnew.t[:NS, :], scalar1=seln.t[:NS, 0:1], scalar2=None, op0=ALU.mult), [pnew, seln], [pnew])
    dve(lambda e: e.tensor_tensor(out=Pn.t[:NS, :, :], in0=pnew.t[:NS, :].unsqueeze(1).to_broadcast([NS, 16, 8]),
                                  in1=idf.t[:NS, 0:NS].unsqueeze(2).to_broadcast([NS, 16, 8]), op=ALU.mult), [pnew, idf], [Pn])

    rdn = s16[3]
    Kg2 = Buf("Kg2", ar[:, 0:4096], [stA])
    Vg2 = Buf("Vg2", ar[:, 4096:8192], [stA])
    KgB, VgB = [Kg, Kg2], [Vg, Vg2]

    def gather_kv(b):
        kd_, vd_ = KgB[b % 2], VgB[b % 2]
        S.op(POOL, lambda e, b=b, kd_=kd_: e.indirect_dma_start(out=kd_.t[:, :], out_offset=None, in_=cache_k[:, :],
                                                                in_offset=bass.IndirectOffsetOnAxis(ap=offs_i.t[:, b:b + 1], axis=0)),
             reads=[offs_i], writes=[kd_], dma_sem=("d", "gk%d" % (b % 2)))
        S.op(POOL, lambda e, b=b, vd_=vd_: e.indirect_dma_start(out=vd_.t[:, :], out_offset=None, in_=cache_v[:, :],
                                                                in_offset=bass.IndirectOffsetOnAxis(ap=offs_i.t[:, b:b + 1], axis=0)),
             reads=[offs_i], writes=[vd_], dma_sem=("d", "gv%d" % (b % 2)))

    gather_kv(0)
    for b in range(NS):
        if b + 1 < NS:
            gather_kv(b + 1)
        Kg, Vg = KgB[b % 2], VgB[b % 2]
        Kv = Kg.t[:, :].rearrange("p (s g d) -> p s g d", s=16, g=2)
        Vv = Vg.t[:, :].rearrange("p (s g d) -> p s g d", s=16, g=2)
        for q4 in range(8):
            pb = nb()
            for i in range(4):
                j = q4 * 4 + i
                S.op(PE, lambda e, pb=pb, i=i, j=j, Kv=Kv: e.transpose(out=pb.t[:, i * 128:(i + 1) * 128], in_=Kv[:, j // 2, j % 2, :],
                                                                   identity=idf.t[:, :]), reads=[Kg, idf], writes=[pb], signal=(i == 3))
            act(KTs.t[:, q4 * 4:(q4 + 1) * 4, :], pb.t[:, :].rearrange("p (j k) -> p j k", j=4), AF.Copy, [pb], [KTs])
        pss = nb()
        for j in range(32):
            g = j % 2
            mm(pss.t[:, j * 4:(j + 1) * 4], KTs.t[:, j, :], QT.t[:, 4 * g:4 * g + 4, b], j == 0, j == 31, [KTs, QT], [pss], j == 31)
        act(PTs.t[:, :, :], pss.t[:, 0:128].rearrange("p (s k) -> p s k", s=16), AF.Exp, [pss], [PTs], scale=ATT_SCALE)
        dve(lambda e, b=b: e.tensor_tensor(out=PTs.t[:, :, :], in0=PTs.t[:, :, :], in1=sel.t[:, b, :].unsqueeze(2).to_broadcast([128, 16, 8]),
                                           op=ALU.mult), [PTs, sel], [PTs])
        dve(lambda e: e.tensor_reduce(out=Pr.t[:, :], in_=PTs.t[:, :, :].rearrange("p s k -> p k s"), axis=AX.X, op=ALU.add), [PTs], [Pr])
        pdn = nb()
        mm(pdn.t[:8, 0:1], Pr.t[:, :], onesf.t[:, 0:1], True, False, [Pr, onesf], [pdn], False)
        mm(pdn.t[:8, 0:1], Pn.t[:NS, b, :], onesf.t[:NS, 0:1], False, True, [Pn, onesf], [pdn], True)
        pso = nb()
        for g in range(2):
            for s_ in range(16):
                mm(pso.t[:8, g * 128:(g + 1) * 128], PTs.t[:, s_, :], Vv[:, s_, g, :], s_ == 0, False, [PTs, Vg], [pso], False)
            mm(pso.t[:8, g * 128:(g + 1) * 128], Pn.t[:NS, b, :], kv_tm.t[:NS, 256 + g * 128:256 + (g + 1) * 128], False, True,
               [Pn, kv_tm], [pso], True)
        dve(lambda e, pdn=pdn: e.reciprocal(out=rdn.t[:8, 0:1], in_=pdn.t[:8, 0:1]), [pdn], [rdn])
        dve(lambda e, pso=pso: e.tensor_scalar(out=o_sb.t[:8, :], in0=pso.t[:8, 0:256], scalar1=rdn.t[:8, 0:1], scalar2=None, op0=ALU.mult),
            [pso, rdn], [o_sb])
        ptt = nb()
        for g in range(2):
            S.op(PE, lambda e, g=g, ptt=ptt: e.transpose(out=ptt.t[:, g * 8:(g + 1) * 8], in_=o_sb.t[:8, g * 128:(g + 1) * 128], identity=idf.t[:8, :8]),
                 reads=[o_sb, idf], writes=[ptt], signal=(g == 1))
        act(attnT.t[:, 0:4, b], ptt.t[:, 0:4], AF.Copy, [ptt], [attnT])
        act(attnT.t[:, 4:8, b], ptt.t[:, 12:16], AF.Copy, [ptt], [attnT])

    proj_tm(attnT, 8, w_attn_out, 0, 1024, copy_out(zci, 0))
    proj_tm(hT, 8, w_in, C_GB, 1024, lambda c0, ncol, pb: act(mt2.t[:, c0:c0 + ncol], pb.t[:NS, :ncol], AF.Sigmoid, [pb], [mt2]))
    dve(lambda e: e.tensor_tensor(out=zci.t[:, :], in0=zci.t[:, :], in1=mt2.t[:, :], op=ALU.mult), [zci, mt2], [zci])
    dve(lambda e: e.tensor_tensor(out=zci.t[:, :], in0=zci.t[:, :], in1=mt1.t[:, :], op=ALU.add), [zci, mt1], [zci])
    tm_to_fm(zci, 8, QT)

    def add_into_xs(c0, ncol, pb):
        dve(lambda e: e.tensor_tensor(out=xs_sb.t[:NS, 0, c0:c0 + ncol], in0=pb.t[:NS, :ncol], in1=xs_sb.t[:NS, 0, c0:c0 + ncol], op=ALU.add),
            [pb, xs_sb], [xs_sb])

    if dbg:
        S.dma(SP, d_mg, zci.t[:, :], reads=[zci], sem="dbg")
        S.dma(SP, d_attn, attnT.t[:, :, 0:NS], reads=[attnT], sem="dbg")
        S.dma(SP, d_ssm, Ssm.t[:, :, :].rearrange("p b s -> p (b s)"), reads=[Ssm], sem="dbg")
        S.dma(SP, d_sel, sel.t[:, :, :].rearrange("p b s -> p (b s)"), reads=[sel], sem="dbg")
        S.dma(SP, d_m16, R3[:, 640:896], reads=m16, sem="dbg")
    proj_tm(QT, 8, w_o, 0, 1024, add_into_xs)
    if dbg:
        S.dma(SP, d_x1, xs_sb.t[:NS, 0, :], reads=[xs_sb], sem="dbg")
    norm_T(xs_sb, 1, NS, gmemT, hT)
    proj_tm(hT, 8, w_mq, 0, 1024, copy_out(q_tm, 0))
    scm, pm, dnm = s16[4], s16[5], s16[6]
    for b in range(NS):
        S.dma(SP, cmk_b.t[:, :, :], cmk_d[b].rearrange("(m p) d -> p m d", p=128), writes=[cmk_b], sem="cm")
        S.dma(SP, cmv_b.t[:, :, :], cmv_d[b].rearrange("(m p) d -> p m d", p=128), writes=[cmv_b], sem="cm")
        for hf in range(2):
            pb = nb()
            mm(pb.t[:, :], idf.t[0:NS, b:b + 1].to_broadcast([NS, 128]), q_tm.t[:NS, hf * 512:(hf + 1) * 512], True, True, [idf, q_tm], [pb], True)
            for m in range(2):
                dve(lambda e, pb=pb, m=m, hf=hf: e.tensor_tensor(out=tmpm.t[:, m, hf * 512:(hf + 1) * 512], in0=cmk_b.t[:, m, hf * 512:(hf + 1) * 512],
                                                                 in1=pb.t[:, :], op=ALU.mult), [cmk_b, pb], [tmpm])
        dve(lambda e: e.tensor_reduce(out=scm.t[:, :], in_=tmpm.t[:, :, :].rearrange("p m (h d) -> p (m h) d", h=4), axis=AX.X, op=ALU.add),
            [tmpm], [scm])
        act(pm.t[:, :], scm.t[:, :], AF.Exp, [scm], [pm], scale=MEM_SCALE)
        pdn = nb()
        mm(pdn.t[:1, 0:8], onesf.t[:, 0:1], pm.t[:, :], True, True, [onesf, pm], [pdn], True)
        dve(lambda e, pdn=pdn: e.tensor_reduce(out=dnm.t[:1, 0:4], in_=pdn.t[:1, 0:8].rearrange("p (m h) -> p h m", m=2), axis=AX.X, op=ALU.add), [pdn], [dnm])
        dve(lambda e: e.reciprocal(out=dnm.t[:1, 0:4], in_=dnm.t[:1, 0:4]), [dnm], [dnm])
        pos = [nb(), nb()]
        for h in range(4):
            for m in range(2):
                mm(pos[h // 2].t[:1, (h % 2) * 256:(h % 2 + 1) * 256], pm.t[:, m * 4 + h: m * 4 + h + 1], cmv_b.t[:, m, h * 256:(h + 1) * 256],
                   m == 0, m == 1, [pm, cmv_b], [pos[h // 2]], m == 1)
        for hp in range(2):
            dve(lambda e, hp=hp, pos=pos: e.tensor_tensor(out=tmpq.t[:1, hp * 512:(hp + 1) * 512].rearrange("p (h d) -> p h d", h=2),
                                                          in0=pos[hp].t[:1, :].rearrange("p (h d) -> p h d", h=2),
                                                          in1=dnm.t[:1, 2 * hp:2 * hp + 2].unsqueeze(2).to_broadcast([1, 2, 256]), op=ALU.mult),
                [pos[hp], dnm], [tmpq])
        S.dma(SP, omtm.t[b:b + 1, :], tmpq.t[0:1, :], reads=[tmpq], writes=[omtm], sem="omt")
    if dbg:
        S.dma(SP, d_om, omtm.t[:NS, :], reads=[omtm], sem="dbg")
    tm_to_fm(omtm, 8, attnT)
    proj_tm(attnT, 8, w_mo, 0, 1024, add_into_xs)
    if dbg:
        S.dma(SP, d_x2, xs_sb.t[:NS, 0, :], reads=[xs_sb], sem="dbg")
    norm_T(xs_sb, 1, NS, gffnT, hT)
    proj_tm(hT, 8, w_gate, 0, DFF, lambda c0, ncol, pb: act(g_tm.t[:, c0:c0 + ncol], pb.t[:NS, :ncol], AF.Silu, [pb], [g_tm]))

    def cons_up(c0, ncol, pb):
        dve(lambda e: e.tensor_tensor(out=g_tm.t[:, c0:c0 + ncol], in0=pb.t[:NS, :ncol], in1=g_tm.t[:, c0:c0 + ncol], op=ALU.mult), [pb, g_tm], [g_tm])
    proj_tm(hT, 8, w_up, 0, DFF, cons_up)
    tm_to_fm(g_tm, 22, fT)
    proj_tm(fT, 22, w_down, 0, 1024, add_into_xs)
    S.dma(SP, gfin.t[:, :], g_final.partition_broadcast(128), writes=[gfin], sem="const")
    rstd_only(xs_sb, 1, NS)
    dve(lambda e: e.scalar_tensor_tensor(out=ysb.t[:NS, :], in0=xs_sb.t[:NS, 0, :], scalar=rstd.t[:NS, 0:1], in1=gfin.t[:NS, :],
                                         op0=ALU.mult, op1=ALU.mult), [xs_sb, rstd, gfin], [ysb])
    S.dma(SP, o_ys, ysb.t[:NS, :], reads=[ysb], sem="o_y")

    S.finish(SP)
    S.replay(ctx)
    ctx.close()
    return nc


_NC_CACHE = {}
_WITH_SAMPLE = [True]
_TRACE = [False]


def kernel(**inp):
    f32 = np.float32
    xp = np.asarray(inp["x_prompt"], f32)
    B = xp.shape[0]
    if "nc" not in _NC_CACHE:
        _NC_CACHE["nc"] = build_program(with_sample=_WITH_SAMPLE[0])
    nc = _NC_CACHE["nc"]
    in_maps = []
    shared = {}
    for name in ("w_in", "conv_w", "w_conv_out", "w_attn_out", "w_o", "w_mq", "w_mk", "w_mv", "w_mo", "w_gate", "w_up", "w_down",
                 "g_mix", "g_mem", "g_mem_kv", "g_ffn"):
        shared[name] = np.ascontiguousarray(np.asarray(inp[name], f32)[0])
    shared["g_final"] = np.ascontiguousarray(np.asarray(inp["g_final"], f32))
    ck_ = np.ascontiguousarray(np.asarray(inp["cache_k"], f32)[0]).reshape(2560 * 8, 4096)
    cv_ = np.ascontiguousarray(np.asarray(inp["cache_v"], f32)[0]).reshape(2560 * 8, 4096)
    cik_ = np.ascontiguousarray(np.asarray(inp["cache_idx_k"], f32)[0]).reshape(2560 * 8, 1024)
    for c in range(8):
        b, j = c // 4, c % 4
        segs = [4 * t + j for t in range(NT)]
        x_own = np.concatenate([xp[b, 512 * s:512 * (s + 1)] for s in segs], axis=0)
        x_halo = np.zeros((8, D), f32)
        for t, s in enumerate(segs):
            if s > 0:
                x_halo[2 * t:2 * t + 2] = xp[b, 512 * s - 2:512 * s]
        qrel = (512.0 * j + np.arange(128, dtype=f32)).reshape(128, 1).astype(f32)
        m = dict(shared)
        m.update({"x_all": np.ascontiguousarray(xp[b]), "x_own": np.ascontiguousarray(x_own), "x_halo": x_halo, "qrel": qrel,
                  "mem": np.ascontiguousarray(np.asarray(inp["mem_prompt"], f32)[b])})
        sl = slice(16 * c, 16 * (c + 1))
        if _WITH_SAMPLE[0]:
          m.update({"xs": np.ascontiguousarray(np.asarray(inp["x_sample"], f32)[sl, 0]),
                  "sconv": np.ascontiguousarray(np.asarray(inp["state_conv"], f32)[0, sl]),
                  "ptab": np.ascontiguousarray(np.asarray(inp["page_table"], np.int32)[sl]),
                  "cache_k": ck_, "cache_v": cv_, "cache_ik": cik_,
                  "cmk": np.ascontiguousarray(np.asarray(inp["cache_mem_k"], f32)[0, sl].reshape(16, 256, D)),
                  "cmv": np.ascontiguousarray(np.asarray(inp["cache_mem_v"], f32)[0, sl].reshape(16, 256, D))})
        in_maps.append(m)
    if _TRACE[0]:
        res = run_bass_kernel_spmd(nc, in_maps, core_ids=list(range(8)), trace=True)
        print("exec_time_ns", res.exec_time_ns)
    else:
        res = run_bass_kernel_spmd(nc, in_maps, core_ids=list(range(8)))
    R = res.results
    y = np.zeros((B, SEQ, D), f32)
    nk = np.zeros((1, B, SEQ, 2, 128), f32)
    nv = np.zeros((1, B, SEQ, 2, 128), f32)
    nik = np.zeros((1, B, SEQ, 64), f32)
    nconv = np.zeros((1, B, 2, D), f32)
    nmk = np.zeros((1, B, 256, 4, 256), f32)
    nmv = np.zeros((1, B, 256, 4, 256), f32)
    for c in range(8):
        b, j = c // 4, c % 4
        for t in range(NT):
            s = 4 * t + j
            sl = slice(512 * s, 512 * (s + 1))
            tl = slice(512 * t, 512 * (t + 1))
            y[b, sl] = R[c]["o_y"][tl]
            nk[0, b, sl] = R[c]["o_k"][tl].reshape(512, 2, 128)
            nv[0, b, sl] = R[c]["o_v"][tl].reshape(512, 2, 128)
            nik[0, b, sl] = R[c]["o_ik"][tl]
        if j == 3:
            nconv[0, b] = R[c]["o_conv"]
        if j == 0:
            nmk[0, b] = R[c]["o_mk"].reshape(256, 4, 256)
            nmv[0, b] = R[c]["o_mv"].reshape(256, 4, 256)
    if not _WITH_SAMPLE[0]:
        return (y, nk, nv, nik, nconv, nmk, nmv)
    ys = np.concatenate([R[c]["o_ys"] for c in range(8)], axis=0).reshape(128, 1, D)
    nks = np.concatenate([R[c]["o_ks"] for c in range(8)], axis=0).reshape(1, 128, 1, 2, 128)
    nvs = np.concatenate([R[c]["o_vs"] for c in range(8)], axis=0).reshape(1, 128, 1, 2, 128)
    niks = np.concatenate([R[c]["o_iks"] for c in range(8)], axis=0).reshape(1, 128, 1, 64)
    ncs = np.concatenate([R[c]["o_convs"] for c in range(8)], axis=0).reshape(1, 128, 2, D)
    return (y, ys, nk, nv, nik, nconv, nmk, nmv, nks, nvs, niks, ncs)
```

```python
from contextlib import ExitStack
import numpy as np
import concourse.bass as bass
import concourse.mybir as mybir
from concourse.bass_utils import run_bass_kernel_spmd

F32 = mybir.dt.float32
BF16 = mybir.dt.bfloat16
I32 = mybir.dt.int32
AF = mybir.ActivationFunctionType
ALU = mybir.AluOpType
AX = mybir.AxisListType

PE, ACT, DVE, POOL, SP = "pe", "act", "dve", "pool", "sp"
COMPUTE = (PE, ACT, DVE, POOL)

D = 1024
SEQ = 8192
NT = 4
TT = 512
MIXW = 6980
C_IN, C_B, C_C, C_Q, C_K, C_V, C_IQ, C_IK, C_IW, C_GA, C_GB = 0, 1024, 2048, 3072, 4096, 4352, 4608, 4864, 4928, 4932, 5956
DFF = 2816
EPS = 1e-6
NBIS = 22
DVE_FRAC = 0.55
PIPE = True
JUNK_BC = True
NEGM = -30000.0
TOPK = 256.0
ATT_SCALE = 128 ** -0.5
MEM_SCALE = 256 ** -0.5


class _St:
    __slots__ = ("last_w", "readers")

    def __init__(self):
        self.last_w = None
        self.readers = {}


class Buf:
    def __init__(self, name, t=None, sts=None):
        self.name = name
        self.t = t
        self.sts = sts if sts is not None else [_St()]

    def alias(self, name, t=None):
        return Buf(name, t if t is not None else self.t, self.sts)


class Sched:
    def __init__(self, nc):
        self.nc = nc
        self.ops = {e: [] for e in (PE, ACT, DVE, POOL, SP)}
        self.sig = {e: 0 for e in COMPUTE}
        self.pending = {e: [] for e in COMPUTE}
        self.known = {e: {} for e in self.ops}
        self.dma_cnt = {}
        self.sems = {}

    def _waits_for(self, eng, ev, waits):
        if ev is None:
            return
        if ev[0] == "c":
            if ev[1] == eng and eng == PE:
                return
            if ev[2][0] is None:
                raise RuntimeError("dependency on unsignaled op %s (consumer %s)" % (ev[1], eng))
            key, val = ("c", ev[1]), ev[2][0]
        else:
            key = ev[1]
            val = self.dma_cnt[key]
        if self.known[eng].get(key, 0) >= val:
            return
        waits[key] = max(waits.get(key, 0), val)

    def op(self, eng, fn, reads=(), writes=(), signal=True, dma_sem=None):
        waits = {}
        for b in reads:
            for st in b.sts:
                self._waits_for(eng, st.last_w, waits)
        for b in writes:
            for st in b.sts:
                self._waits_for(eng, st.last_w, waits)
                for r in st.readers.values():
                    self._waits_for(eng, r, waits)
        for k, v in waits.items():
            self.known[eng][k] = v
        if dma_sem is not None:
            self.dma_cnt[dma_sem] = self.dma_cnt.get(dma_sem, 0) + 16
            ev = ("d", dma_sem, self.dma_cnt[dma_sem])
            inc = (dma_sem, 16)
        else:
            box = [None]
            ev = ("c", eng, box)
            self.pending[eng].append(box)
            inc = None
            if signal:
                self.sig[eng] += 1
                for bx in self.pending[eng]:
                    bx[0] = self.sig[eng]
                self.pending[eng] = []
                inc = (("c", eng), 1)
        rk = ev[1]
        for b in reads:
            for st in b.sts:
                st.readers[rk] = ev
        for b in writes:
            for st in b.sts:
                st.last_w = ev
                st.readers = {}
        self.ops[eng].append((list(waits.items()), fn, inc))
        return ev

    def dma(self, queue, out_ap, in_ap, reads=(), writes=(), sem=None, **kw):
        return self.op(queue, lambda e: e.dma_start(out=out_ap, in_=in_ap, **kw),
                       reads=reads, writes=writes, dma_sem=("d", sem))

    def finish(self, eng=SP):
        waits = [(k, v) for k, v in self.dma_cnt.items() if self.known[eng].get(k, 0) < v]
        self.ops[eng].append((waits, None, None))

    def replay(self, ctx):
        nc = self.nc
        keys = set()
        for e in self.ops:
            for waits, fn, inc in self.ops[e]:
                for k, _ in waits:
                    keys.add(k)
                if inc:
                    keys.add(inc[0])
        for k in sorted(keys, key=str):
            self.sems[k] = ctx.enter_context(nc.semaphore("s_" + "_".join(str(x) for x in k)))
        block = ctx.enter_context(nc.Block())
        engmap = {PE: block.tensor, ACT: block.scalar, DVE: block.vector, POOL: block.gpsimd, SP: block.sync}
        for e, deco in engmap.items():
            oplist = self.ops[e]
            if not oplist:
                continue

            def body(engobj, oplist=oplist):
                for waits, fn, inc in oplist:
                    for k, v in waits:
                        engobj.wait_ge(self.sems[k], v)
                    if fn is None:
                        continue
                    ins = fn(engobj)
                    if inc:
                        ins.then_inc(self.sems[inc[0]], inc[1])
            deco(body)


def build_program(with_sample=True, do_prompt=True, dbg=False):
    nc = bass.Bass("TRN2", target_bir_lowering=False)

    def din(name, shape, dt=F32):
        return nc.dram_tensor(name, list(shape), dt, kind="ExternalInput").ap()

    def dout(name, shape, dt=F32):
        return nc.dram_tensor(name, list(shape), dt, kind="ExternalOutput").ap()

    x_all = din("x_all", [SEQ, D])
    x_own = din("x_own", [NT * TT, D])
    x_halo = din("x_halo", [8, D])
    qrel_d = din("qrel", [128, 1])
    mem_d = din("mem", [256, D])
    w_in = din("w_in", [D, MIXW])
    conv_w = din("conv_w", [3, D])
    w_conv_out = din("w_conv_out", [D, D])
    w_attn_out = din("w_attn_out", [D, D])
    w_o = din("w_o", [D, D])
    w_mq = din("w_mq", [D, D])
    w_mk = din("w_mk", [D, D])
    w_mv = din("w_mv", [D, D])
    w_mo = din("w_mo", [D, D])
    w_gate = din("w_gate", [D, DFF])
    w_up = din("w_up", [D, DFF])
    w_down = din("w_down", [DFF, D])
    g_mix = din("g_mix", [D])
    g_mem = din("g_mem", [D])
    g_mem_kv = din("g_mem_kv", [D])
    g_ffn = din("g_ffn", [D])
    g_final = din("g_final", [D])

    if with_sample:
        xs_d = din("xs", [16, D])
        sconv_d = din("sconv", [16, 2, D])
        ptab_d = din("ptab", [16, 16], I32)
        cache_k = din("cache_k", [2560 * 8, 4096])
        cache_v = din("cache_v", [2560 * 8, 4096])
        cache_ik = din("cache_ik", [2560 * 8, 1024])
        cmk_d = din("cmk", [16, 256, D])
        cmv_d = din("cmv", [16, 256, D])
        o_ys = dout("o_ys", [16, D])
        o_ks = dout("o_ks", [16, 256])
        o_vs = dout("o_vs", [16, 256])
        o_iks = dout("o_iks", [16, 64])
        o_convs = dout("o_convs", [16, 2, D])

    if dbg:
        d_x1 = dout("d_x1", [16, D]); d_x2 = dout("d_x2", [16, D]); d_attn = dout("d_attn", [128, 8, 16], BF16); d_mg = dout("d_mg", [16, D])
        d_ssm = dout("d_ssm", [128, 256]); d_sel = dout("d_sel", [128, 256]); d_m16 = dout("d_m16", [128, 256]); d_om = dout("d_om", [16, D]); d_offs = dout("d_offs", [128, 16], I32); d_ikg = dout("d_ikg", [128, 1024]); d_rep = dout("d_rep", [128, 324])
    o_y = dout("o_y", [NT * TT, D])
    o_k = dout("o_k", [NT * TT, 256])
    o_v = dout("o_v", [NT * TT, 256])
    o_ik = dout("o_ik", [NT * TT, 64])
    o_conv = dout("o_conv", [2, D])
    o_mk = dout("o_mk", [256, D])
    o_mv = dout("o_mv", [256, D])

    ctx = ExitStack()
    S = Sched(nc)

    def sb(name, shape, dt):
        return Buf(name, ctx.enter_context(nc.sbuf_tensor(name, list(shape), dt)))

    KT = sb("KT", [128, 2, SEQ], BF16)
    Vb = sb("Vb", [128, 64, 256], BF16)
    ikT = sb("ikT", [128, SEQ], BF16)
    NWB = 3
    wb = [sb("wb%d" % i, [128, 8, 512], BF16) for i in range(NWB)]
    hT = sb("hT", [128, 8, TT], BF16)
    QT = sb("QT", [128, 8, TT], BF16)
    iqT = sb("iqT", [128, 2, TT], BF16)
    maT = sb("maT", [128, 8, TT], BF16)
    attnT = sb("attnT", [128, 8, TT], BF16)
    arena_t = ctx.enter_context(nc.sbuf_tensor("arena", [128, 12288], F32))
    hn = sb("hn", [128, D], BF16)
    hn2 = sb("hn2", [128, D], BF16)
    jkS = sb("jkS", [128, 2], BF16)
    PTb = [sb("PT%d" % i, [128, 512], BF16) for i in range(2)]
    ident = sb("ident", [128, 128], BF16)
    ones = sb("ones", [128, 128], BF16)
    gmixT = sb("gmixT", [128, 8], F32)
    gmemT = sb("gmemT", [128, 8], F32)
    gmkvT = sb("gmkvT", [128, 8], F32)
    gffnT = sb("gffnT", [128, 8], F32)
    cwT = sb("cwT", [128, 8, 3], F32)
    qrel = sb("qrel_sb", [128, 1], F32)
    negc = sb("negc", [128, 1664], BF16)
    ck = sb("ck", [128, NBIS], F32)
    sk = sb("sk", [128, NBIS], F32)
    small = sb("small", [128, 64], F32)
    epsb = sb("epsb", [128, 1], F32)
    iw_sb = sb("iw_sb", [128, 4, 4], F32)
    uhalo = sb("uhalo", [128, 8, 8], F32)
    memKT = sb("memKT", [128, 8, 256], BF16)
    memV = sb("memV", [128, 2, D], BF16)
    rden = sb("rden", [128, 512], F32)
    jk = sb("jk", [128, 2], BF16)
    jkA = sb("jkA", [128, 2], BF16)

    ar = arena_t
    stA, stB = _St(), _St()

    def av(name, ap, lo, hi):
        sts = ([stA] if lo < 8192 else []) + ([stB] if hi > 8192 else [])
        return Buf(name, ap, sts)

    xin = av("xin", ar[:, 0:4096].rearrange("p (b d) -> p b d", b=4), 0, 4096)
    ccT = av("ccT", ar[:, 4096:6144].rearrange("p (c n) -> p c n", c=4), 4096, 6144)
    uT = av("uT", ar[:, 6144:8200].rearrange("p (c n) -> p c n", c=4), 6144, 8200)
    cvT = av("cvT", ar[:, 8200:10248].rearrange("p (c n) -> p c n", c=4), 8200, 10248)
    ostg = av("ostg", ar[:, 10248:10824], 10248, 10824)
    Sidx = av("Sidx", ar[:, 0:8192], 0, 8192)
    maskb = av("maskb", ar[:, 8192:12288].bitcast(BF16), 8192, 12288)
    m_a = av("m_a", ar[:, 0:4096].rearrange("p (c n) -> p c n", c=8), 0, 4096)
    tmpA = av("tmpA", ar[:, 4096:4608], 4096, 4608)
    tmpB = av("tmpB", ar[:, 4608:5120], 4608, 5120)
    fT = av("fT", ar[:, 4096:9728].bitcast(BF16).rearrange("p (c n) -> p c n", c=22), 4096, 9728)
    ysb = av("ysb", ar[:, 9728:10752], 9728, 10752)
    gfin = av("gfin", ar[:, 10752:11776], 10752, 11776)
    tmpC = av("tmpC", ar[:, 11776:12288], 11776, 12288)
    xm = av("xm", ar[:, 0:2048].rearrange("p (b d) -> p b d", b=2), 0, 2048)
    xh = av("xh", ar[:, 0:1024].rearrange("p (b d) -> p b d", b=1), 0, 1024)

    banks = [Buf("pb%d" % i, ctx.enter_context(nc.psum_tensor("pb%d" % i, [128, 512], F32))) for i in range(8)]
    GEN = banks[:6]
    po = [banks[6], banks[6]]
    pden = [banks[7], banks[7]]
    rr = [0]

    def nb():
        b = GEN[rr[0] % len(GEN)]
        rr[0] += 1
        return b

    def mm(out_ap, lhsT, rhs, st, sp, R, W, sig):
        S.op(PE, lambda e: e.matmul(out_ap, lhsT=lhsT, rhs=rhs, start=st, stop=sp), reads=R, writes=W, signal=sig)

    def act(out_ap, in_ap, func, R, W, **kw):
        S.op(ACT, lambda e: e.activation(out=out_ap, in_=in_ap, func=func, **kw), reads=R, writes=W)

    def dve(fn, R, W):
        S.op(DVE, fn, reads=R, writes=W)

    wrr = [0]

    def wload(src_ap, kc, ncols):
        b = wb[wrr[0] % NWB]
        wrr[0] += 1
        S.dma(POOL, b.t[:, 0:kc, 0:ncols], src_ap.rearrange("(c p) n -> p c n", p=128), writes=[b], sem=b.name)
        return b

    S.op(POOL, lambda e: e.memset(ident.t[:], 0.0), writes=[ident])
    S.op(POOL, lambda e: e.affine_select(out=ident.t[:], in_=ident.t[:], pattern=[[-1, 128]], compare_op=ALU.not_equal,
                                         fill=1.0, base=0, channel_multiplier=1), reads=[ident], writes=[ident])
    I4ap = ident.t[:, :].unsqueeze(1).to_broadcast([128, 4, 128])
    S.op(POOL, lambda e: e.memset(ones.t[:], 1.0), writes=[ones])
    S.op(POOL, lambda e: e.memset(epsb.t[:], EPS), writes=[epsb])
    for k in range(NBIS):
        S.op(POOL, lambda e, k=k: e.memset(ck.t[:, k:k + 1], 2.0 * (1.0 + 1e-6) / 2.0 ** (k + 1)), writes=[ck])
    for g_d, g_s in ((g_mix, gmixT), (g_mem, gmemT), (g_mem_kv, gmkvT), (g_ffn, gffnT)):
        S.dma(SP, g_s.t[:], g_d.rearrange("(c p) -> p c", p=128), writes=[g_s], sem="const", allow_slow_non_contiguous=True)
    for j in range(3):
        S.dma(SP, cwT.t[:, :, j], conv_w[j, :].rearrange("(c p) -> p c", p=128), writes=[cwT], sem="const", allow_slow_non_contiguous=True)
    S.dma(SP, qrel.t[:], qrel_d, writes=[qrel], sem="const")
    S.op(POOL, lambda e: e.iota(Sidx.t[:, 0:1664], pattern=[[1, 1664]], base=0, channel_multiplier=0,
                                allow_small_or_imprecise_dtypes=True), writes=[Sidx])
    dve(lambda e: e.tensor_scalar(out=negc.t[:], in0=Sidx.t[:, 0:1664], scalar1=qrel.t[:, 0:1], scalar2=-1e30,
                                  op0=ALU.is_gt, op1=ALU.mult), [Sidx, qrel], [negc])

    ssq = small.alias("ssq", small.t[:, 0:4])
    rstd = small.alias("rstd", small.t[:, 4:8])

    def rstd_only(xt, nblk, P):
        for blk in range(nblk):
            act(jkS.t[:P, 0:1].to_broadcast([P, D]), xt.t[:P, blk, :], AF.Square, [xt], [jkS, ssq], accum_out=ssq.t[:P, blk:blk + 1])
            act(rstd.t[:P, blk:blk + 1], ssq.t[:P, blk:blk + 1], AF.Sqrt, [ssq, epsb], [rstd], scale=1.0 / D, bias=epsb.t[:P, 0:1])
            dve(lambda e, blk=blk: e.reciprocal(out=rstd.t[:P, blk:blk + 1], in_=rstd.t[:P, blk:blk + 1]), [rstd], [rstd])

    def norm_T(xt, nblk, P, gT, out_hT, col0=0):
        rstd_only(xt, nblk, P)
        for blk in range(nblk):
            hb = hn if blk % 2 == 0 else hn2
            dve(lambda e, blk=blk, hb=hb: e.tensor_scalar(out=hb.t[:P, :], in0=xt.t[:P, blk, :], scalar1=rstd.t[:P, blk:blk + 1],
                                                         scalar2=None, op0=ALU.mult), [xt, rstd], [hb])
            pb = nb()
            pv = pb.t[:, :].bitcast(BF16).rearrange("p (c n) -> p c n", c=8)
            for c in range(8):
                S.op(PE, lambda e, c=c, pv=pv, hb=hb: e.transpose(out=pv[:, c, :P], in_=hb.t[:P, c * 128:(c + 1) * 128],
                                                                  identity=ident.t[:P, :P]),
                     reads=[hb, ident], writes=[pb], signal=(c == 7))
            dve(lambda e, blk=blk, pv=pv: e.tensor_tensor(
                out=out_hT.t[:, :, col0 + blk * P: col0 + (blk + 1) * P], in0=pv[:, :, :P],
                in1=gT.t[:, :].unsqueeze(2).to_broadcast([128, 8, P]), op=ALU.mult), [pb, gT], [out_hT])

    def proj_fm(wbuf, wcol0, nchunks_out, rhsT, kc, N, consume, rcol0=0):
        for j in range(nchunks_out):
            pb = nb()
            for c in range(kc):
                mm(pb.t[:, :N], wbuf.t[:, c, wcol0 + j * 128: wcol0 + (j + 1) * 128], rhsT.t[:, c, rcol0:rcol0 + N],
                   c == 0, c == kc - 1, [wbuf, rhsT], [pb], c == kc - 1)
            consume(j, pb)

    S.dma(SP, xm.t[:, :, :], mem_d.rearrange("(b p) d -> p b d", p=128), writes=[xm], sem="xin")
    norm_T(xm, 2, 128, gmkvT, hT)
    for wd, od, is_k in ((w_mk, o_mk, True), (w_mv, o_mv, False)):
        for half in range(2):
            wbuf = wload(wd[:, half * 512:(half + 1) * 512], 8, 512)
            for blk in range(2):
                pb = nb()
                for c in range(8):
                    mm(pb.t[:, :], hT.t[:, c, blk * 128:(blk + 1) * 128], wbuf.t[:, c, :], c == 0, c == 7, [hT, wbuf], [pb], c == 7)
                act(tmpA.t[:, :], pb.t[:, :], AF.Copy, [pb], [tmpA])
                S.dma(SP, od[blk * 128:(blk + 1) * 128, half * 512:(half + 1) * 512], tmpA.t[:, :], reads=[tmpA], sem="o_small")
                if not is_k:
                    dve(lambda e, pb=pb, blk=blk, half=half: e.tensor_copy(out=memV.t[:, blk, half * 512:(half + 1) * 512], in_=pb.t[:, :]),
                        [pb], [memV])
            if is_k:
                def cons(j, pb, half=half):
                    act(memKT.t[:, half * 4 + j, :], pb.t[:, :256], AF.Copy, [pb], [memKT])
                proj_fm(wbuf, 0, 4, hT, 8, 256, cons)

    S.dma(SP, xh.t[:8, 0, :], x_halo, writes=[xh], sem="xin")
    norm_T(xh, 1, 8, gmixT, hT)
    cch = small.alias("cch", small.t[:, 16:24])
    for half in range(2):
        wcc = wload(w_in[:, C_C + half * 512: C_C + (half + 1) * 512], 8, 512)
        wci = wload(w_in[:, C_IN + half * 512: C_IN + (half + 1) * 512], 8, 512)
        for j in range(4):
            pb = nb()
            for c in range(8):
                mm(pb.t[:, :8], wcc.t[:, c, j * 128:(j + 1) * 128], hT.t[:, c, 0:8], c == 0, c == 7, [wcc, hT], [pb], c == 7)
            act(cch.t[:, :], pb.t[:, :8], AF.Copy, [pb], [cch])
            pb2 = nb()
            for c in range(8):
                mm(pb2.t[:, :8], wci.t[:, c, j * 128:(j + 1) * 128], hT.t[:, c, 0:8], c == 0, c == 7, [wci, hT], [pb2], c == 7)
            dve(lambda e, pb2=pb2, j=j, half=half: e.tensor_tensor(out=uhalo.t[:, half * 4 + j, :], in0=pb2.t[:, :8], in1=cch.t[:, :],
                                                                  op=ALU.mult), [pb2, cch], [uhalo])

    wkv = wload(w_in[:, C_K:C_K + 512], 8, 512)
    wik = wb[wrr[0] % NWB]
    wrr[0] += 1
    for hh in range(2):
        S.dma(POOL, wik.t[:, :, hh * 64:(hh + 1) * 64], w_in[:, C_IK:C_IK + 64].rearrange("(c p) n -> p c n", p=128),
              writes=[wik], sem=wik.name)
    xinB = Buf("xinB", ar[:, 4096:8192].rearrange("p (b d) -> p b d", b=4))
    S.op(DVE, lambda e: e.memset(small.t[:, 42:43], 0.0), reads=[], writes=[xinB, ccT])
    xbufs = [xin, xinB]

    def load_x(tile):
        xb = xbufs[tile % 2]
        S.dma(SP, xb.t[:, :, :], x_all[tile * TT:(tile + 1) * TT, :].rearrange("(b p) d -> p b d", p=128), writes=[xb],
              sem="xin%d" % (tile % 2))

    if do_prompt:
        load_x(0)
    for tile in range(SEQ // TT if do_prompt else 0):
        if tile + 1 < SEQ // TT:
            load_x(tile + 1)
        norm_T(xbufs[tile % 2], 4, 128, gmixT, hT)
        for g in range(2):
            pb = nb()
            for c in range(8):
                mm(pb.t[:, :], wkv.t[:, c, g * 128:(g + 1) * 128], hT.t[:, c, :], c == 0, c == 7, [wkv, hT], [pb], c == 7)
            act(KT.t[:, g, tile * TT:(tile + 1) * TT], pb.t[:, :], AF.Copy, [pb], [KT])
        pb = nb()
        for c in range(8):
            mm(pb.t[:, :], wik.t[:, c, 0:128], hT.t[:, c, :], c == 0, c == 7, [wik, hT], [pb], c == 7)
        act(ikT.t[:, tile * TT:(tile + 1) * TT], pb.t[:, :], AF.Copy, [pb], [ikT])
        for blk in range(4):
            pb = nb()
            for c in range(8):
                mm(pb.t[:, :256], hT.t[:, c, blk * 128:(blk + 1) * 128], wkv.t[:, c, 256:512], c == 0, c == 7, [hT, wkv], [pb], c == 7)
            dve(lambda e, pb=pb, tile=tile, blk=blk: e.tensor_copy(out=Vb.t[:, tile * 4 + blk, :], in_=pb.t[:, :256]), [pb], [Vb])

    S.op(DVE, lambda e: e.memset(small.t[:, 43:44], 0.0), reads=[], writes=[xinB, ccT, uT])
    Bq = small.alias("Bq", small.t[:, 8:9])
    cand = small.alias("cand", small.t[:, 9:10])
    cnt = small.alias("cnt", small.t[:, 10:11])
    btmp = small.alias("btmp", small.t[:, 11:12])
    ssA = Buf("ssA", small.t[:, 12:13])

    for t in range(NT if do_prompt else 0):
        tok0 = t * TT
        S.dma(SP, xin.t[:, :, :], x_own[tok0:tok0 + TT, :].rearrange("(b p) d -> p b d", p=128), writes=[xin], sem="xin")
        norm_T(xin, 4, 128, gmixT, hT)
        wkv = wload(w_in[:, C_K:C_K + 512], 8, 512)
        wsm = wload(w_in[:, C_IK:C_IK + 68], 8, 68)
        for blk in range(4):
            pb = nb()
            for c in range(8):
                mm(pb.t[:, :], hT.t[:, c, blk * 128:(blk + 1) * 128], wkv.t[:, c, :], c == 0, c == 7, [hT, wkv], [pb], c == 7)
            act(ostg.t[:, 0:512], pb.t[:, :], AF.Copy, [pb], [ostg])
            pb2 = nb()
            for c in range(8):
                mm(pb2.t[:, :68], hT.t[:, c, blk * 128:(blk + 1) * 128], wsm.t[:, c, 0:68], c == 0, c == 7, [hT, wsm], [pb2], c == 7)
            dve(lambda e, pb2=pb2: e.tensor_copy(out=ostg.t[:, 512:576], in_=pb2.t[:, 0:64]), [pb2], [ostg])
            dve(lambda e, pb2=pb2, blk=blk: e.tensor_copy(out=iw_sb.t[:, blk, :], in_=pb2.t[:, 64:68]), [pb2], [iw_sb])
            r0 = tok0 + blk * 128
            S.dma(SP, o_k[r0:r0 + 128, :], ostg.t[:, 0:256], reads=[ostg], sem="o_small")
            S.dma(SP, o_v[r0:r0 + 128, :], ostg.t[:, 256:512], reads=[ostg], sem="o_small")
            S.dma(SP, o_ik[r0:r0 + 128, :], ostg.t[:, 512:576], reads=[ostg], sem="o_small")
        wq = wload(w_in[:, C_IQ:C_IQ + 256], 8, 256)
        proj_fm(wq, 0, 2, hT, 8, TT, lambda j, pb: act(iqT.t[:, j, :], pb.t[:, :], AF.Copy, [pb], [iqT]))
        for half in range(2):
            wq = wload(w_in[:, C_Q + half * 512: C_Q + (half + 1) * 512], 8, 512)
            proj_fm(wq, 0, 4, hT, 8, TT,
                    lambda j, pb, half=half: act(QT.t[:, half * 4 + j, :], pb.t[:, :], AF.Copy, [pb], [QT]))
        for half in range(2):
            wcc = wload(w_in[:, C_C + half * 512: C_C + (half + 1) * 512], 8, 512)
            proj_fm(wcc, 0, 4, hT, 8, TT, lambda j, pb: act(ccT.t[:, j, :], pb.t[:, :], AF.Copy, [pb], [ccT]))
            wci = wload(w_in[:, C_IN + half * 512: C_IN + (half + 1) * 512], 8, 512)

            def cons_u(j, pb, half=half, t=t):
                dve(lambda e: e.tensor_tensor(out=uT.t[:, j, 2:514], in0=pb.t[:, :], in1=ccT.t[:, j, :], op=ALU.mult), [pb, ccT], [uT])
                dve(lambda e: e.tensor_copy(out=uT.t[:, j, 0:2], in_=uhalo.t[:, half * 4 + j, 2 * t:2 * t + 2]), [uhalo], [uT])
            proj_fm(wci, 0, 4, hT, 8, TT, cons_u)
            wcb = wload(w_in[:, C_B + half * 512: C_B + (half + 1) * 512], 8, 512)
            for j in range(4):
                ch = half * 4 + j
                dve(lambda e, j=j, ch=ch: e.tensor_scalar(out=cvT.t[:, j, :], in0=uT.t[:, j, 2:514], scalar1=cwT.t[:, ch, 2:3],
                                                          scalar2=None, op0=ALU.mult), [uT, cwT], [cvT])
                dve(lambda e, j=j, ch=ch: e.scalar_tensor_tensor(out=cvT.t[:, j, :], in0=uT.t[:, j, 1:513], scalar=cwT.t[:, ch, 1:2],
                                                                 in1=cvT.t[:, j, :], op0=ALU.mult, op1=ALU.add), [uT, cwT, cvT], [cvT])
                dve(lambda e, j=j, ch=ch: e.scalar_tensor_tensor(out=cvT.t[:, j, :], in0=uT.t[:, j, 0:512], scalar=cwT.t[:, ch, 0:1],
                                                                 in1=cvT.t[:, j, :], op0=ALU.mult, op1=ALU.add), [uT, cwT, cvT], [cvT])
            if t == NT - 1:
                for tk in range(2):
                    S.dma(SP, o_conv[tk, half * 512:(half + 1) * 512].rearrange("(c p) -> p c", p=128), uT.t[:, :, 512 + tk],
                          reads=[uT], sem="o_small", allow_slow_non_contiguous=True)

            def cons_b(j, pb, half=half):
                dve(lambda e: e.tensor_tensor(out=maT.t[:, half * 4 + j, :], in0=pb.t[:, :], in1=cvT.t[:, j, :], op=ALU.mult), [pb, cvT], [maT])
            proj_fm(wcb, 0, 4, hT, 8, TT, cons_b)

        def geom(r):
            nkb = 16 * t + 13 + r
            return nkb, nkb * 128, (16 * t + r) * 128, slice(r * 128, (r + 1) * 128)

        def idx_phase(r):
            nkb, nk, kbase, qs = geom(r)
            for k0 in range(0, nk, 512):
                w = min(512, nk - k0)
                pbs = []
                for h in range(4):
                    pb = nb()
                    pbs.append(pb)
                    hp = slice((h % 2) * 64, (h % 2) * 64 + 64)
                    mm(pb.t[:, :w], iqT.t[hp, h // 2, qs], ikT.t[hp, k0:k0 + w], True, True, [iqT, ikT], [pb], True)
                dve(lambda e, pb=pbs[0], k0=k0, w=w, r=r: e.tensor_scalar(out=Sidx.t[:, k0:k0 + w], in0=pb.t[:, :w], scalar1=0.0,
                                                                          scalar2=iw_sb.t[:, r, 0:1], op0=ALU.max, op1=ALU.mult),
                    [pbs[0], iw_sb], [Sidx])
                for h in range(1, 4):
                    act(pbs[h].t[:, :w], pbs[h].t[:, :w], AF.Relu, [pbs[h]], [pbs[h]])
                    dve(lambda e, pb=pbs[h], h=h, k0=k0, w=w, r=r: e.scalar_tensor_tensor(
                        out=Sidx.t[:, k0:k0 + w], in0=pb.t[:, :w], scalar=iw_sb.t[:, r, h:h + 1], in1=Sidx.t[:, k0:k0 + w],
                        op0=ALU.mult, op1=ALU.add), [pbs[h], iw_sb, Sidx], [Sidx])
            dve(lambda e, nk=nk: e.tensor_reduce(out=Bq.t[:, :], in_=Sidx.t[:, 0:nk], axis=AX.X, op=ALU.max,
                                                apply_absolute_value=True), [Sidx], [Bq])
            dve(lambda e, kbase=kbase, nk=nk: e.tensor_tensor(out=Sidx.t[:, kbase:nk], in0=Sidx.t[:, kbase:nk], in1=negc.t[:, 0:nk - kbase],
                                                             op=ALU.add), [Sidx, negc], [Sidx])
            dve(lambda e: e.tensor_scalar(out=sk.t[:, :], in0=ck.t[:, :], scalar1=Bq.t[:, 0:1], scalar2=None, op0=ALU.mult), [ck, Bq], [sk])
            dve(lambda e: e.scalar_tensor_tensor(out=cand.t[:, :], in0=Bq.t[:, :], scalar=-1.0, in1=sk.t[:, 0:1], op0=ALU.mult, op1=ALU.add),
                [Bq, sk], [cand])

        def bis_iter(r, k):
            nkb, nk, kbase, qs = geom(r)
            kd = max(1, int(round(nkb * DVE_FRAC))) * 128
            na = nk - kd
            dve(lambda e, kd=kd: e.tensor_scalar(out=(jk.t[:, 0:1].to_broadcast([128, kd]) if JUNK_BC else maskb.t[:, 0:kd]), in0=Sidx.t[:, 0:kd], scalar1=cand.t[:, 0:1], scalar2=None,
                                                op0=ALU.is_ge, op1=ALU.add, accum_out=cnt.t[:, 0:1]), [Sidx, cand], [jk if JUNK_BC else maskb, cnt])
            if na > 0:
                act(jkA.t[:, 0:1].to_broadcast([128, na]), Sidx.t[:, kd:nk], AF.Sign, [Sidx, cand], [jkA, ssA], scale=-1.0, bias=cand.t[:, 0:1],
                    accum_out=ssA.t[:, 0:1])
                dve(lambda e: e.scalar_tensor_tensor(out=cnt.t[:, :], in0=ssA.t[:, :], scalar=-0.5, in1=cnt.t[:, :], op0=ALU.mult, op1=ALU.add),
                    [ssA, cnt], [cnt])
            last = (k == NBIS - 1)
            dve(lambda e, last=last, na=na: e.tensor_scalar(out=btmp.t[:, :], in0=cnt.t[:, :], scalar1=TOPK - 0.5 * na, scalar2=(1.0 if last else 0.5),
                                                           op0=ALU.is_ge, op1=ALU.subtract), [cnt], [btmp])
            dve(lambda e, k=k: e.scalar_tensor_tensor(out=cand.t[:, :], in0=btmp.t[:, :], scalar=sk.t[:, k:k + 1], in1=cand.t[:, :],
                                                     op0=ALU.mult, op1=ALU.add), [btmp, sk, cand], [cand])

        def mask_phase(r):
            nkb, nk, kbase, qs = geom(r)
            dve(lambda e, nk=nk: e.tensor_scalar(out=maskb.t[:, 0:nk], in0=Sidx.t[:, 0:nk], scalar1=cand.t[:, 0:1], scalar2=NEGM,
                                                op0=ALU.is_lt, op1=ALU.mult), [Sidx, cand], [maskb])

        def attn_gen(r):
            nkb, nk, kbase, qs = geom(r)
            for g in range(2):
                pend = None

                def pv_part(kb, pt, g=g):
                    mm(po[g].t[:, :], Vb.t[:, kb, g * 128:(g + 1) * 128], pt.t[:, :], kb == 0, kb == nkb - 1, [Vb, pt], [po[g]], kb == nkb - 1)
                    mm(pden[g].t[:, :], ones.t[:, :], pt.t[:, :], kb == 0, kb == nkb - 1, [ones, pt], [pden[g]], kb == nkb - 1)

                for kb in range(nkb):
                    pb = nb()
                    mm(pb.t[:, :], KT.t[:, g, kb * 128:(kb + 1) * 128], QT.t[:, 4 * g:4 * g + 4, qs], True, False, [KT, QT], [pb], False)
                    mm(pb.t[:, :], maskb.t[:, kb * 128:(kb + 1) * 128], I4ap, False, True, [maskb, ident], [pb], True)
                    pt = PTb[kb % 2]
                    act(pt.t[:, :], pb.t[:, :], AF.Exp, [pb], [pt], scale=ATT_SCALE)
                    if pend is not None:
                        pv_part(*pend)
                        yield
                    pend = (kb, pt)
                pv_part(*pend)
                yield
                dve(lambda e, g=g: e.reciprocal(out=rden.t[:, :], in_=pden[g].t[:, :]), [pden[g]], [rden])
                dve(lambda e, g=g, qs=qs: e.tensor_tensor(out=attnT.t[:, 4 * g:4 * g + 4, qs], in0=po[g].t[:, :].rearrange("p (h q) -> p h q", h=4),
                                                         in1=rden.t[:, :].rearrange("p (h q) -> p h q", h=4), op=ALU.mult),
                    [po[g], rden], [attnT])

        if not PIPE:
            for r in range(4):
                idx_phase(r)
                for k in range(NBIS):
                    bis_iter(r, k)
                mask_phase(r)
                for _ in attn_gen(r):
                    pass
        else:
          idx_phase(0)
          for k in range(NBIS):
            bis_iter(0, k)
          mask_phase(0)
        for r in range(4 if PIPE else 0):
            gen = attn_gen(r)
            if r + 1 < 4:
                idx_phase(r + 1)
                nunits = 2 * geom(r)[0]
                per = -(-nunits // NBIS)
                alive = True
                for k in range(NBIS):
                    for _ in range(per):
                        if alive and next(gen, "done") == "done":
                            alive = False
                    bis_iter(r + 1, k)
                for _ in gen:
                    pass
                mask_phase(r + 1)
            else:
                for _ in gen:
                    pass

        mgT = QT
        for half in range(2):
            wga = wload(w_in[:, C_GA + half * 512: C_GA + (half + 1) * 512], 8, 512)
            wco = wload(w_conv_out[:, half * 512:(half + 1) * 512], 8, 512)
            for j in range(4):
                ch = half * 4 + j
                js = slice(j * 128, (j + 1) * 128)
                pga, pa = nb(), nb()
                for c in range(8):
                    mm(pga.t[:, :], wga.t[:, c, js], hT.t[:, c, :], c == 0, c == 7, [wga, hT], [pga], c == 7)
                for c in range(8):
                    mm(pa.t[:, :], wco.t[:, c, js], maT.t[:, c, :], c == 0, c == 7, [wco, maT], [pa], c == 7)
                act(tmpA.t[:, :], pga.t[:, :], AF.Sigmoid, [pga], [tmpA])
                dve(lambda e, pa=pa, ch=ch: e.tensor_tensor(out=m_a.t[:, ch, :], in0=pa.t[:, :], in1=tmpA.t[:, :], op=ALU.mult), [pa, tmpA], [m_a])
        for half in range(2):
            wgb = wload(w_in[:, C_GB + half * 512: C_GB + (half + 1) * 512], 8, 512)
            wao = wload(w_attn_out[:, half * 512:(half + 1) * 512], 8, 512)
            for j in range(4):
                ch = half * 4 + j
                js = slice(j * 128, (j + 1) * 128)
                pgb, pbb = nb(), nb()
                for c in range(8):
                    mm(pgb.t[:, :], wgb.t[:, c, js], hT.t[:, c, :], c == 0, c == 7, [wgb, hT], [pgb], c == 7)
                for c in range(8):
                    mm(pbb.t[:, :], wao.t[:, c, js], attnT.t[:, c, :], c == 0, c == 7, [wao, attnT], [pbb], c == 7)
                act(tmpB.t[:, :], pgb.t[:, :], AF.Sigmoid, [pgb], [tmpB])
                dve(lambda e, pbb=pbb: e.tensor_tensor(out=tmpB.t[:, :], in0=pbb.t[:, :], in1=tmpB.t[:, :], op=ALU.mult), [pbb, tmpB], [tmpB])
                dve(lambda e, ch=ch: e.tensor_tensor(out=mgT.t[:, ch, :], in0=m_a.t[:, ch, :], in1=tmpB.t[:, :], op=ALU.add), [m_a, tmpB], [mgT])
        S.dma(SP, xin.t[:, :, :], x_own[tok0:tok0 + TT, :].rearrange("(b p) d -> p b d", p=128), writes=[xin], sem="xin")

        def resid_add(actT, kc_total, wsrc):
            for half in range(2):
                pbs = [nb() for _ in range(4)]
                kdone = 0
                while kdone < kc_total:
                    kc = min(8, kc_total - kdone)
                    wbuf = wload(wsrc[kdone * 128:(kdone + kc) * 128, half * 512:(half + 1) * 512], kc, 512)
                    for blk in range(4):
                        for c in range(kc):
                            first = (kdone + c == 0)
                            lastc = (kdone + c == kc_total - 1)
                            mm(pbs[blk].t[:, :], actT.t[:, kdone + c, blk * 128:(blk + 1) * 128], wbuf.t[:, c, :], first, lastc,
                               [actT, wbuf], [pbs[blk]], lastc)
                    kdone += kc
                for blk in range(4):
                    dve(lambda e, blk=blk, half=half, pb=pbs[blk]: e.tensor_tensor(
                        out=xin.t[:, blk, half * 512:(half + 1) * 512], in0=pb.t[:, :], in1=xin.t[:, blk, half * 512:(half + 1) * 512],
                        op=ALU.add), [pbs[blk], xin], [xin])

        resid_add(mgT, 8, w_o)
        norm_T(xin, 4, 128, gmemT, hT)
        qmT = QT
        for half in range(2):
            wq = wload(w_mq[:, half * 512:(half + 1) * 512], 8, 512)
            proj_fm(wq, 0, 4, hT, 8, TT, lambda j, pb, half=half: act(qmT.t[:, half * 4 + j, :], pb.t[:, :], AF.Copy, [pb], [qmT]))
        omT = attnT
        for h in range(4):
            pts = []
            for kblk in range(2):
                pb = nb()
                for dd in range(2):
                    mm(pb.t[:, :], memKT.t[:, 2 * h + dd, kblk * 128:(kblk + 1) * 128], qmT.t[:, 2 * h + dd, :], dd == 0, dd == 1,
                       [memKT, qmT], [pb], dd == 1)
                pt = PTb[kblk]
                act(pt.t[:, :], pb.t[:, :], AF.Exp, [pb], [pt], scale=MEM_SCALE)
                pts.append(pt)
            for kblk in range(2):
                mm(pden[0].t[:, :], ones.t[:, :], pts[kblk].t[:, :], kblk == 0, kblk == 1, [ones, pts[kblk]], [pden[0]], kblk == 1)
            dve(lambda e: e.reciprocal(out=rden.t[:, :], in_=pden[0].t[:, :]), [pden[0]], [rden])
            for dd in range(2):
                pb = nb()
                for kblk in range(2):
                    mm(pb.t[:, :], memV.t[:, kblk, (2 * h + dd) * 128:(2 * h + dd + 1) * 128], pts[kblk].t[:, :], kblk == 0, kblk == 1,
                       [memV, pts[kblk]], [pb], kblk == 1)
                dve(lambda e, pb=pb, h=h, dd=dd: e.tensor_tensor(out=omT.t[:, 2 * h + dd, :], in0=pb.t[:, :], in1=rden.t[:, :], op=ALU.mult),
                    [pb, rden], [omT])
        resid_add(omT, 8, w_mo)
        norm_T(xin, 4, 128, gffnT, hT)
        for p0 in range(0, DFF, 512):
            ncol = min(512, DFF - p0)
            wg = wload(w_gate[:, p0:p0 + ncol], 8, ncol)
            wu = wload(w_up[:, p0:p0 + ncol], 8, ncol)
            for j in range(ncol // 128):
                fc = p0 // 128 + j
                js = slice(j * 128, (j + 1) * 128)
                pg, pu = nb(), nb()
                for c in range(8):
                    mm(pg.t[:, :], wg.t[:, c, js], hT.t[:, c, :], c == 0, c == 7, [wg, hT], [pg], c == 7)
                for c in range(8):
                    mm(pu.t[:, :], wu.t[:, c, js], hT.t[:, c, :], c == 0, c == 7, [wu, hT], [pu], c == 7)
                act(tmpC.t[:, :], pg.t[:, :], AF.Silu, [pg], [tmpC])
                dve(lambda e, pu=pu, fc=fc: e.tensor_tensor(out=fT.t[:, fc, :], in0=pu.t[:, :], in1=tmpC.t[:, :], op=ALU.mult), [pu, tmpC], [fT])
        resid_add(fT, 22, w_down)
        S.dma(SP, gfin.t[:, :], g_final.partition_broadcast(128), writes=[gfin], sem="const")
        rstd_only(xin, 4, 128)
        for blk in range(4):
            dve(lambda e, blk=blk: e.scalar_tensor_tensor(out=ysb.t[:, :], in0=xin.t[:, blk, :], scalar=rstd.t[:, blk:blk + 1], in1=gfin.t[:, :],
                                                         op0=ALU.mult, op1=ALU.mult), [xin, rstd, gfin], [ysb])
            r0 = tok0 + blk * 128
            S.dma(SP, o_y[r0:r0 + 128, :], ysb.t[:, :], reads=[ysb], sem="o_y")


    if not with_sample:
        S.finish(SP)
        S.replay(ctx)
        ctx.close()
        return nc
    NS = 16
    NBS = 30
    R1 = KT.t[:, :, :].rearrange("p g k -> p (g k)").bitcast(F32)
    R2 = Vb.t[:, :, :].rearrange("p k c -> p (k c)").bitcast(F32)
    R3 = ikT.t[:, :].bitcast(F32)
    views = []

    def sv(name, ap):
        b = Buf(name, ap)
        views.append(b)
        return b

    Kg = sv("Kg", R1[:, 0:4096])
    Vg = sv("Vg", R1[:, 4096:8192])
    KTs = sv("KTs", R2[:, 0:2048].bitcast(BF16).rearrange("p (j k) -> p j k", j=32))
    ikg = sv("ikg", R2[:, 2048:3072])
    tmpi = sv("tmpi", R2[:, 3072:4096])
    q_tm = sv("q_tm", R2[:, 4096:5120])
    kv_tm = sv("kv_tm", R2[:, 5120:5632])
    smt = sv("smt", R2[:, 5632:5956])
    xs_sb = sv("xs_sb", R2[:, 6144:7168].rearrange("p (b d) -> p b d", b=1))
    omtm = sv("omtm", R2[:, 7168:8192])
    Ssm = sv("Ssm", R3[:, 0:256].rearrange("p (b s) -> p b s", b=16))
    sel = sv("sel", R3[:, 256:512].rearrange("p (b s) -> p b s", b=16))
    cmpb = sv("cmpb", R3[:, 512:640].bitcast(BF16).rearrange("p (b s) -> p b s", b=16))
    m16 = [sv("m16_%d" % i, R3[:, 640 + 16 * i: 656 + 16 * i]) for i in range(16)]
    cntb = sv("cntb", R3[:, 896:912])
    PTs = sv("PTs", R3[:, 1024:1152].rearrange("p (s k) -> p s k", s=16))
    rep_sb = sv("rep_sb", R3[:, 1152:1476])
    o_sb = sv("o_sb", R3[:, 1536:1792])
    Pr = sv("Pr", R3[:, 1792:1800])
    Pn = sv("Pn", R3[:, 1800:1928].rearrange("p (b k) -> p b k", b=16))
    pnew = sv("pnew", R3[:, 1928:1936])
    s16 = [sv("s16_%d" % i, R3[:, 1936 + 8 * i: 1944 + 8 * i]) for i in range(8)]
    offs_f = sv("offs_f", R3[:, 2048:2064])
    offs_i = sv("offs_i", R3[:, 2064:2080].bitcast(I32))
    ptT = sv("ptT", R3[:, 2080:2096].bitcast(I32))
    ptTf = sv("ptTf", R3[:, 2096:2112])
    pm8 = sv("pm8", R3[:, 2112:2113])
    idf = sv("idf", R3[:, 2176:2304])
    onesf = sv("onesf", R3[:, 2304:2432])
    Amat = sv("Amat", R3[:, 2432:2560])
    tmpq = sv("tmpq", R3[:, 2560:3584])
    cmk_b = Kg.alias("cmk_b", R1[:, 0:2048].rearrange("p (m d) -> p m d", m=2))
    cmv_b = Kg.alias("cmv_b", R1[:, 2048:4096].rearrange("p (m d) -> p m d", m=2))
    tmpm = Vg.alias("tmpm", R1[:, 4096:6144].rearrange("p (m d) -> p m d", m=2))
    S.op(DVE, lambda e: e.memset(small.t[:, 40:41], 0.0), reads=[], writes=[KT, Vb, ikT] + views)

    zci = av("zci", ar[:16, 0:1024], 0, 1024)
    zcc = av("zcc", ar[:16, 1024:2048], 1024, 2048)
    zcb = av("zcb", ar[:16, 2048:3072], 2048, 3072)
    prev = av("prev", ar[:16, 3072:5120].rearrange("p (j d) -> p j d", j=2), 3072, 5120)
    cwb = av("cwb", ar[:16, 5120:8192].rearrange("p (j d) -> p j d", j=3), 5120, 8192)
    mt1 = av("mt1", ar[:16, 8192:9216], 8192, 9216)
    mt2 = av("mt2", ar[:16, 9216:10240], 9216, 10240)
    g_tm = av("g_tm", ar[:16, 0:2816], 0, 2816)
    u_tm = av("u_tm", ar[:16, 2816:5632], 2816, 5632)

    def proj_tm(actT, kc_total, wsrc, col_lo, ncols_total, consume):
        for c0 in range(0, ncols_total, 512):
            ncol = min(512, ncols_total - c0)
            pb = nb()
            kdone = 0
            while kdone < kc_total:
                kc = min(8, kc_total - kdone)
                wbuf = wload(wsrc[kdone * 128:(kdone + kc) * 128, col_lo + c0: col_lo + c0 + ncol], kc, ncol)
                for c in range(kc):
                    first = (kdone + c == 0)
                    lastc = (kdone + c == kc_total - 1)
                    mm(pb.t[:NS, :ncol], actT.t[:, kdone + c, 0:NS], wbuf.t[:, c, :ncol], first, lastc, [actT, wbuf], [pb], lastc)
                kdone += kc
            consume(c0, ncol, pb)

    def tm_to_fm(src, nchunks, dst):
        for c0 in range(0, nchunks, 8):
            n = min(8, nchunks - c0)
            dve(lambda e, c0=c0, n=n: e.tensor_copy(out=hn.t[:NS, 0:n * 128], in_=src.t[:NS, c0 * 128:(c0 + n) * 128]), [src], [hn])
            pb = nb()
            pv = pb.t[:, :].bitcast(BF16).rearrange("p (c n) -> p c n", c=8)
            for c in range(n):
                S.op(PE, lambda e, c=c, pv=pv: e.transpose(out=pv[:, c, :NS], in_=hn.t[:NS, c * 128:(c + 1) * 128], identity=ident.t[:NS, :NS]),
                     reads=[hn, ident], writes=[pb], signal=(c == n - 1))
            act(dst.t[:, c0:c0 + n, 0:NS], pv[:, 0:n, :NS], AF.Copy, [pb], [dst])

    def copy_out(dstv, col0):
        return lambda c0, ncol, pb: act(dstv.t[:NS, col0 + c0: col0 + c0 + ncol], pb.t[:NS, :ncol], AF.Copy, [pb], [dstv])

    S.op(POOL, lambda e: e.memset(idf.t[:, :], 0.0), writes=[idf])
    S.op(POOL, lambda e: e.affine_select(out=idf.t[:, :], in_=idf.t[:, :], pattern=[[-1, 128]], compare_op=ALU.not_equal,
                                         fill=1.0, base=0, channel_multiplier=1), reads=[idf], writes=[idf])
    S.op(POOL, lambda e: e.memset(onesf.t[:, :], 1.0), writes=[onesf])
    S.op(POOL, lambda e: e.memset(Amat.t[:, :], 1.0), writes=[Amat])
    S.op(POOL, lambda e: e.affine_select(out=Amat.t[:NS, :], in_=Amat.t[:NS, :], pattern=[[1, 128]], compare_op=ALU.is_ge,
                                         fill=0.0, base=0, channel_multiplier=-8), reads=[Amat], writes=[Amat])
    S.op(POOL, lambda e: e.affine_select(out=Amat.t[:NS, :], in_=Amat.t[:NS, :], pattern=[[-1, 128]], compare_op=ALU.is_ge,
                                         fill=0.0, base=7, channel_multiplier=8), reads=[Amat], writes=[Amat])
    S.op(POOL, lambda e: e.iota(pm8.t[:, :], pattern=[[0, 1]], base=0, channel_multiplier=1, allow_small_or_imprecise_dtypes=True), writes=[pm8])
    pbo = nb()
    mm(pbo.t[:, 16:17], Amat.t[:NS, :], pm8.t[:NS, :], True, True, [Amat, pm8], [pbo], True)
    dve(lambda e, pbo=pbo: e.scalar_tensor_tensor(out=pm8.t[:, :], in0=pbo.t[:, 16:17], scalar=-8.0, in1=pm8.t[:, :], op0=ALU.mult, op1=ALU.add),
        [pbo, pm8], [pm8])
    S.dma(SP, ptT.t[:NS, :], ptab_d.rearrange("b n -> n b"), writes=[ptT], sem="const", allow_slow_non_contiguous=True)
    dve(lambda e: e.tensor_copy(out=ptTf.t[:NS, :], in_=ptT.t[:NS, :]), [ptT], [ptTf])
    pbo = nb()
    mm(pbo.t[:, :NS], Amat.t[:NS, :], ptTf.t[:NS, :], True, True, [Amat, ptTf], [pbo], True)
    dve(lambda e, pbo=pbo: e.tensor_scalar(out=offs_f.t[:, :], in0=pbo.t[:, :NS], scalar1=8.0, scalar2=pm8.t[:, 0:1], op0=ALU.mult, op1=ALU.add),
        [pbo, pm8], [offs_f])
    dve(lambda e: e.tensor_copy(out=offs_i.t[:, :], in_=offs_f.t[:, :]), [offs_f], [offs_i])

    S.dma(SP, xs_sb.t[:NS, 0, :], xs_d, writes=[xs_sb], sem="xin")
    norm_T(xs_sb, 1, NS, gmixT, hT)
    proj_tm(hT, 8, w_in, C_K, 512, copy_out(kv_tm, 0))
    proj_tm(hT, 8, w_in, C_IQ, 324, copy_out(smt, 0))
    proj_tm(hT, 8, w_in, C_Q, 1024, copy_out(q_tm, 0))
    S.dma(SP, o_ks, kv_tm.t[:NS, 0:256], reads=[kv_tm], sem="o_small")
    S.dma(SP, o_vs, kv_tm.t[:NS, 256:512], reads=[kv_tm], sem="o_small")
    S.dma(SP, o_iks, smt.t[:NS, 256:320], reads=[smt], sem="o_small")
    tm_to_fm(q_tm, 8, QT)
    proj_tm(hT, 8, w_in, C_IN, 1024, copy_out(zci, 0))
    proj_tm(hT, 8, w_in, C_C, 1024, copy_out(zcc, 0))
    proj_tm(hT, 8, w_in, C_B, 1024, copy_out(zcb, 0))
    S.dma(SP, prev.t[:, :, :], sconv_d, writes=[prev], sem="xin")
    for j in range(3):
        S.dma(SP, cwb.t[:, j, :], conv_w[j, :].partition_broadcast(NS), writes=[cwb], sem="xin")
    dve(lambda e: e.tensor_tensor(out=zci.t[:, :], in0=zci.t[:, :], in1=zcc.t[:, :], op=ALU.mult), [zci, zcc], [zci])
    S.dma(SP, o_convs[:, 0, :], prev.t[:, 1, :], reads=[prev], sem="o_small")
    S.dma(SP, o_convs[:, 1, :], zci.t[:, :], reads=[zci], sem="o_small")
    dve(lambda e: e.tensor_tensor(out=zcc.t[:, :], in0=zci.t[:, :], in1=cwb.t[:, 2, :], op=ALU.mult), [zci, cwb], [zcc])
    dve(lambda e: e.tensor_tensor(out=mt1.t[:, :], in0=prev.t[:, 1, :], in1=cwb.t[:, 1, :], op=ALU.mult), [prev, cwb], [mt1])
    dve(lambda e: e.tensor_tensor(out=zcc.t[:, :], in0=zcc.t[:, :], in1=mt1.t[:, :], op=ALU.add), [zcc, mt1], [zcc])
    dve(lambda e: e.tensor_tensor(out=mt1.t[:, :], in0=prev.t[:, 0, :], in1=cwb.t[:, 0, :], op=ALU.mult), [prev, cwb], [mt1])
    dve(lambda e: e.tensor_tensor(out=zcc.t[:, :], in0=zcc.t[:, :], in1=mt1.t[:, :], op=ALU.add), [zcc, mt1], [zcc])
    dve(lambda e: e.tensor_tensor(out=zcb.t[:, :], in0=zcb.t[:, :], in1=zcc.t[:, :], op=ALU.mult), [zcb, zcc], [zcb])
    tm_to_fm(zcb, 8, maT)
    proj_tm(maT, 8, w_conv_out, 0, 1024, copy_out(mt1, 0))
    proj_tm(hT, 8, w_in, C_GA, 1024, lambda c0, ncol, pb: act(mt2.t[:, c0:c0 + ncol], pb.t[:NS, :ncol], AF.Sigmoid, [pb], [mt2]))
    dve(lambda e: e.tensor_tensor(out=mt1.t[:, :], in0=mt1.t[:, :], in1=mt2.t[:, :], op=ALU.mult), [mt1, mt2], [mt1])

    snew, snew_rep, Bs, candS, totS, bt1, bt2, tauS, am, selnr = m16[0], m16[1], m16[2], m16[3], m16[4], m16[5], m16[6], m16[7], m16[8], m16[9]
    ikg2 = av("ikg2", ar[:, 11264:12288], 11264, 12288)
    ikgB = [ikg, ikg2]

    def gather_ik(b):
        dst = ikgB[b % 2]
        S.op(POOL, lambda e, b=b, dst=dst: e.indirect_dma_start(out=dst.t[:, :], out_offset=None, in_=cache_ik[:, :],
                                                                in_offset=bass.IndirectOffsetOnAxis(ap=offs_i.t[:, b:b + 1], axis=0)),
             reads=[offs_i], writes=[dst], dma_sem=("d", "gik%d" % (b % 2)))

    gather_ik(0)
    for b in range(NS):
        if b + 1 < NS:
            gather_ik(b + 1)
        ikg = ikgB[b % 2]
        pb = nb()
        mm(pb.t[:, :324], idf.t[0:NS, b:b + 1].to_broadcast([NS, 128]), smt.t[:NS, :], True, True, [idf, smt], [pb], True)
        act(rep_sb.t[:, :], pb.t[:, :324], AF.Copy, [pb], [rep_sb])
        for h in range(4):
            dve(lambda e, h=h, ikg=ikg: e.tensor_tensor(out=tmpi.t[:, :].rearrange("p (s d) -> p s d", s=16),
                                               in0=ikg.t[:, :].rearrange("p (s d) -> p s d", s=16),
                                               in1=rep_sb.t[:, h * 64:(h + 1) * 64].unsqueeze(1).to_broadcast([128, 16, 64]), op=ALU.mult),
                [ikg, rep_sb], [tmpi])
            dve(lambda e: e.tensor_reduce(out=bt1.t[:, :], in_=tmpi.t[:, :].rearrange("p (s d) -> p s d", s=16), axis=AX.X, op=ALU.add),
                [tmpi], [bt1])
            if h == 0:
                dve(lambda e, b=b: e.tensor_scalar(out=Ssm.t[:, b, :], in0=bt1.t[:, :], scalar1=0.0, scalar2=rep_sb.t[:, 320:321],
                                                   op0=ALU.max, op1=ALU.mult), [bt1, rep_sb], [Ssm])
            else:
                dve(lambda e, h=h: e.tensor_scalar(out=bt1.t[:, :], in0=bt1.t[:, :], scalar1=0.0, scalar2=rep_sb.t[:, 320 + h:321 + h],
                                                   op0=ALU.max, op1=ALU.mult), [bt1, rep_sb], [bt1])
                dve(lambda e, b=b: e.tensor_tensor(out=Ssm.t[:, b, :], in0=Ssm.t[:, b, :], in1=bt1.t[:, :], op=ALU.add), [Ssm, bt1], [Ssm])
    if dbg:
        S.dma(SP, d_offs, offs_i.t[:, :], reads=[offs_i], sem="dbg")
        S.dma(SP, d_ikg, ikg.t[:, :], reads=[ikg], sem="dbg")
        S.dma(SP, d_rep, rep_sb.t[:, :], reads=[rep_sb], sem="dbg")
    dve(lambda e: e.tensor_tensor(out=tmpq.t[:NS, 0:256].rearrange("p (h d) -> p h d", h=4), in0=smt.t[:NS, 0:256].rearrange("p (h d) -> p h d", h=4),
                                  in1=smt.t[:NS, 256:320].unsqueeze(1).to_broadcast([NS, 4, 64]), op=ALU.mult), [smt], [tmpq])
    dve(lambda e: e.tensor_reduce(out=s16[0].t[:NS, 0:4], in_=tmpq.t[:NS, 0:256].rearrange("p (h d) -> p h d", h=4), axis=AX.X, op=ALU.add),
        [tmpq], [s16[0]])
    dve(lambda e: e.tensor_scalar(out=s16[0].t[:NS, 0:4], in0=s16[0].t[:NS, 0:4], scalar1=0.0, scalar2=None, op0=ALU.max), [s16[0]], [s16[0]])
    dve(lambda e: e.tensor_tensor(out=s16[0].t[:NS, 0:4], in0=s16[0].t[:NS, 0:4], in1=smt.t[:NS, 320:324], op=ALU.mult), [s16[0], smt], [s16[0]])
    dve(lambda e: e.tensor_reduce(out=snew.t[:NS, 0:1], in_=s16[0].t[:NS, 0:4], axis=AX.X, op=ALU.add), [s16[0]], [snew])

    def rep_diag(src_col, dst):
        dve(lambda e: e.tensor_scalar(out=bt2.t[:NS, :], in0=idf.t[:NS, 0:NS], scalar1=src_col, scalar2=None, op0=ALU.mult), [idf, snew, s16[1]], [bt2])
        pbx = nb()
        mm(pbx.t[:, :NS], onesf.t[:NS, :], bt2.t[:NS, :], True, True, [onesf, bt2], [pbx], True)
        dve(lambda e: e.tensor_copy(out=dst.t[:, :], in_=pbx.t[:, :NS]), [pbx], [dst])

    rep_diag(snew.t[:NS, 0:1], snew_rep)
    dve(lambda e: e.tensor_reduce(out=am.t[:, :], in_=Ssm.t[:, :, :], axis=AX.X, op=ALU.max, apply_absolute_value=True), [Ssm], [am])
    dve(lambda e: e.tensor_reduce(out=bt1.t[:, :], in_=snew_rep.t[:, :].unsqueeze(2), axis=AX.X, op=ALU.max, apply_absolute_value=True), [snew_rep], [bt1])
    dve(lambda e: e.tensor_tensor(out=am.t[:, :], in0=am.t[:, :], in1=bt1.t[:, :], op=ALU.add), [am, bt1], [am])
    pbx = nb()
    mm(pbx.t[:, :NS], onesf.t[:, :], am.t[:, :], True, True, [onesf, am], [pbx], True)
    dve(lambda e, pbx=pbx: e.tensor_copy(out=Bs.t[:, :], in_=pbx.t[:, :NS]), [pbx], [Bs])
    c0k = 2.0 * (1.0 + 1e-6) / 2.0
    dve(lambda e: e.tensor_scalar(out=candS.t[:, :], in0=Bs.t[:, :], scalar1=c0k - 1.0, scalar2=None, op0=ALU.mult), [Bs], [candS])
    for k in range(NBS):
        ckk = 2.0 * (1.0 + 1e-6) / 2.0 ** (k + 1)
        dve(lambda e: e.tensor_tensor(out=cmpb.t[:, :, :], in0=Ssm.t[:, :, :], in1=candS.t[:, :].unsqueeze(2).to_broadcast([128, 16, 16]),
                                      op=ALU.is_ge), [Ssm, candS], [cmpb])
        dve(lambda e: e.tensor_reduce(out=cntb.t[:, :], in_=cmpb.t[:, :, :], axis=AX.X, op=ALU.add), [cmpb], [cntb])
        pbx = nb()
        mm(pbx.t[:, :NS], onesf.t[:, :], cntb.t[:, :], True, True, [onesf, cntb], [pbx], True)
        dve(lambda e: e.tensor_tensor(out=bt1.t[:, :], in0=snew_rep.t[:, :], in1=candS.t[:, :], op=ALU.is_ge), [snew_rep, candS], [bt1])
        dve(lambda e, pbx=pbx: e.tensor_tensor(out=totS.t[:, :], in0=pbx.t[:, :NS], in1=bt1.t[:, :], op=ALU.add), [pbx, bt1], [totS])
        last = (k == NBS - 1)
        dve(lambda e, last=last: e.tensor_scalar(out=bt1.t[:, :], in0=totS.t[:, :], scalar1=TOPK, scalar2=(1.0 if last else 0.5),
                                                op0=ALU.is_ge, op1=ALU.subtract), [totS], [bt1])
        dve(lambda e: e.tensor_tensor(out=bt1.t[:, :], in0=bt1.t[:, :], in1=Bs.t[:, :], op=ALU.mult), [bt1, Bs], [bt1])
        dve(lambda e, ckk=ckk: e.scalar_tensor_tensor(out=candS.t[:, :], in0=bt1.t[:, :], scalar=ckk, in1=candS.t[:, :], op0=ALU.mult, op1=ALU.add),
            [bt1, candS], [candS])
    dve(lambda e: e.tensor_tensor(out=sel.t[:, :, :], in0=Ssm.t[:, :, :], in1=candS.t[:, :].unsqueeze(2).to_broadcast([128, 16, 16]), op=ALU.is_ge),
        [Ssm, candS], [sel])
    dve(lambda e: e.tensor_tensor(out=selnr.t[:, :], in0=snew_rep.t[:, :], in1=candS.t[:, :], op=ALU.is_ge), [snew_rep, candS], [selnr])
    dve(lambda e: e.tensor_tensor(out=bt2.t[:NS, :], in0=selnr.t[:NS, :], in1=idf.t[:NS, 0:NS], op=ALU.mult), [selnr, idf], [bt2])
    seln = s16[2]
    dve(lambda e: e.tensor_reduce(out=seln.t[:NS, 0:1], in_=bt2.t[:NS, :], axis=AX.X, op=ALU.add), [bt2], [seln])
    dve(lambda e: e.tensor_tensor(out=tmpq.t[:NS, :].rearrange("p (g h d) -> p g h d", g=2, h=4),
                                  in0=q_tm.t[:NS, :].rearrange("p (g h d) -> p g h d", g=2, h=4),
                                  in1=kv_tm.t[:NS, 0:256].rearrange("p (g d) -> p g d", g=2).unsqueeze(2).to_broadcast([NS, 2, 4, 128]),
                                  op=ALU.mult), [q_tm, kv_tm], [tmpq])
    dve(lambda e: e.tensor_reduce(out=pnew.t[:NS, :], in_=tmpq.t[:NS, :].rearrange("p (k d) -> p k d", k=8), axis=AX.X, op=ALU.add), [tmpq], [pnew])
    act(pnew.t[:NS, :], pnew.t[:NS, :], AF.Exp, [pnew], [pnew], scale=ATT_SCALE)
    dve(lambda e: e.tensor_scalar(out=pnew.t[:NS, :], in0=pnew.t[:NS, :], scalar1=seln.t[:NS, 0:1], scalar2=None, op0=ALU.mult), [pnew, seln], [pnew])
    dve(lambda e: e.tensor_tensor(out=Pn.t[:NS, :, :], in0=pnew.t[:NS, :].unsqueeze(1).to_broadcast([NS, 16, 8]),
                                  in1=idf.t[:NS, 0:NS].unsqueeze(2).to_broadcast([NS, 16, 8]), op=ALU.mult), [pnew, idf], [Pn])

    rdn = s16[3]
    Kg2 = Buf("Kg2", ar[:, 0:4096], [stA])
    Vg2 = Buf("Vg2", ar[:, 4096:8192], [stA])
    KgB, VgB = [Kg, Kg2], [Vg, Vg2]

    def gather_kv(b):
        kd_, vd_ = KgB[b % 2], VgB[b % 2]
        S.op(POOL, lambda e, b=b, kd_=kd_: e.indirect_dma_start(out=kd_.t[:, :], out_offset=None, in_=cache_k[:, :],
                                                                in_offset=bass.IndirectOffsetOnAxis(ap=offs_i.t[:, b:b + 1], axis=0)),
             reads=[offs_i], writes=[kd_], dma_sem=("d", "gk%d" % (b % 2)))
        S.op(POOL, lambda e, b=b, vd_=vd_: e.indirect_dma_start(out=vd_.t[:, :], out_offset=None, in_=cache_v[:, :],
                                                                in_offset=bass.IndirectOffsetOnAxis(ap=offs_i.t[:, b:b + 1], axis=0)),
             reads=[offs_i], writes=[vd_], dma_sem=("d", "gv%d" % (b % 2)))

    gather_kv(0)
    for b in range(NS):
        if b + 1 < NS:
            gather_kv(b + 1)
        Kg, Vg = KgB[b % 2], VgB[b % 2]
        Kv = Kg.t[:, :].rearrange("p (s g d) -> p s g d", s=16, g=2)
        Vv = Vg.t[:, :].rearrange("p (s g d) -> p s g d", s=16, g=2)
        for q4 in range(8):
            pb = nb()
            for i in range(4):
                j = q4 * 4 + i
                S.op(PE, lambda e, pb=pb, i=i, j=j, Kv=Kv: e.transpose(out=pb.t[:, i * 128:(i + 1) * 128], in_=Kv[:, j // 2, j % 2, :],
                                                                   identity=idf.t[:, :]), reads=[Kg, idf], writes=[pb], signal=(i == 3))
            act(KTs.t[:, q4 * 4:(q4 + 1) * 4, :], pb.t[:, :].rearrange("p (j k) -> p j k", j=4), AF.Copy, [pb], [KTs])
        pss = nb()
        for j in range(32):
            g = j % 2
            mm(pss.t[:, j * 4:(j + 1) * 4], KTs.t[:, j, :], QT.t[:, 4 * g:4 * g + 4, b], j == 0, j == 31, [KTs, QT], [pss], j == 31)
        act(PTs.t[:, :, :], pss.t[:, 0:128].rearrange("p (s k) -> p s k", s=16), AF.Exp, [pss], [PTs], scale=ATT_SCALE)
        dve(lambda e, b=b: e.tensor_tensor(out=PTs.t[:, :, :], in0=PTs.t[:, :, :], in1=sel.t[:, b, :].unsqueeze(2).to_broadcast([128, 16, 8]),
                                           op=ALU.mult), [PTs, sel], [PTs])
        dve(lambda e: e.tensor_reduce(out=Pr.t[:, :], in_=PTs.t[:, :, :].rearrange("p s k -> p k s"), axis=AX.X, op=ALU.add), [PTs], [Pr])
        pdn = nb()
        mm(pdn.t[:8, 0:1], Pr.t[:, :], onesf.t[:, 0:1], True, False, [Pr, onesf], [pdn], False)
        mm(pdn.t[:8, 0:1], Pn.t[:NS, b, :], onesf.t[:NS, 0:1], False, True, [Pn, onesf], [pdn], True)
        pso = nb()
        for g in range(2):
            for s_ in range(16):
                mm(pso.t[:8, g * 128:(g + 1) * 128], PTs.t[:, s_, :], Vv[:, s_, g, :], s_ == 0, False, [PTs, Vg], [pso], False)
            mm(pso.t[:8, g * 128:(g + 1) * 128], Pn.t[:NS, b, :], kv_tm.t[:NS, 256 + g * 128:256 + (g + 1) * 128], False, True,
               [Pn, kv_tm], [pso], True)
        dve(lambda e, pdn=pdn: e.reciprocal(out=rdn.t[:8, 0:1], in_=pdn.t[:8, 0:1]), [pdn], [rdn])
        dve(lambda e, pso=pso: e.tensor_scalar(out=o_sb.t[:8, :], in0=pso.t[:8, 0:256], scalar1=rdn.t[:8, 0:1], scalar2=None, op0=ALU.mult),
            [pso, rdn], [o_sb])
        ptt = nb()
        for g in range(2):
            S.op(PE, lambda e, g=g, ptt=ptt: e.transpose(out=ptt.t[:, g * 8:(g + 1) * 8], in_=o_sb.t[:8, g * 128:(g + 1) * 128], identity=idf.t[:8, :8]),
                 reads=[o_sb, idf], writes=[ptt], signal=(g == 1))
        act(attnT.t[:, 0:4, b], ptt.t[:, 0:4], AF.Copy, [ptt], [attnT])
        act(attnT.t[:, 4:8, b], ptt.t[:, 12:16], AF.Copy, [ptt], [attnT])

    proj_tm(attnT, 8, w_attn_out, 0, 1024, copy_out(zci, 0))
    proj_tm(hT, 8, w_in, C_GB, 1024, lambda c0, ncol, pb: act(mt2.t[:, c0:c0 + ncol], pb.t[:NS, :ncol], AF.Sigmoid, [pb], [mt2]))
    dve(lambda e: e.tensor_tensor(out=zci.t[:, :], in0=zci.t[:, :], in1=mt2.t[:, :], op=ALU.mult), [zci, mt2], [zci])
    dve(lambda e: e.tensor_tensor(out=zci.t[:, :], in0=zci.t[:, :], in1=mt1.t[:, :], op=ALU.add), [zci, mt1], [zci])
    tm_to_fm(zci, 8, QT)

    def add_into_xs(c0, ncol, pb):
        dve(lambda e: e.tensor_tensor(out=xs_sb.t[:NS, 0, c0:c0 + ncol], in0=pb.t[:NS, :ncol], in1=xs_sb.t[:NS, 0, c0:c0 + ncol], op=ALU.add),
            [pb, xs_sb], [xs_sb])

    if dbg:
        S.dma(SP, d_mg, zci.t[:, :], reads=[zci], sem="dbg")
        S.dma(SP, d_attn, attnT.t[:, :, 0:NS], reads=[attnT], sem="dbg")
        S.dma(SP, d_ssm, Ssm.t[:, :, :].rearrange("p b s -> p (b s)"), reads=[Ssm], sem="dbg")
        S.dma(SP, d_sel, sel.t[:, :, :].rearrange("p b s -> p (b s)"), reads=[sel], sem="dbg")
        S.dma(SP, d_m16, R3[:, 640:896], reads=m16, sem="dbg")
    proj_tm(QT, 8, w_o, 0, 1024, add_into_xs)
    if dbg:
        S.dma(SP, d_x1, xs_sb.t[:NS, 0, :], reads=[xs_sb], sem="dbg")
    norm_T(xs_sb, 1, NS, gmemT, hT)
    proj_tm(hT, 8, w_mq, 0, 1024, copy_out(q_tm, 0))
    scm, pm, dnm = s16[4], s16[5], s16[6]
    for b in range(NS):
        S.dma(SP, cmk_b.t[:, :, :], cmk_d[b].rearrange("(m p) d -> p m d", p=128), writes=[cmk_b], sem="cm")
        S.dma(SP, cmv_b.t[:, :, :], cmv_d[b].rearrange("(m p) d -> p m d", p=128), writes=[cmv_b], sem="cm")
        for hf in range(2):
            pb = nb()
            mm(pb.t[:, :], idf.t[0:NS, b:b + 1].to_broadcast([NS, 128]), q_tm.t[:NS, hf * 512:(hf + 1) * 512], True, True, [idf, q_tm], [pb], True)
            for m in range(2):
                dve(lambda e, pb=pb, m=m, hf=hf: e.tensor_tensor(out=tmpm.t[:, m, hf * 512:(hf + 1) * 512], in0=cmk_b.t[:, m, hf * 512:(hf + 1) * 512],
                                                                 in1=pb.t[:, :], op=ALU.mult), [cmk_b, pb], [tmpm])
        dve(lambda e: e.tensor_reduce(out=scm.t[:, :], in_=tmpm.t[:, :, :].rearrange("p m (h d) -> p (m h) d", h=4), axis=AX.X, op=ALU.add),
            [tmpm], [scm])
        act(pm.t[:, :], scm.t[:, :], AF.Exp, [scm], [pm], scale=MEM_SCALE)
        pdn = nb()
        mm(pdn.t[:1, 0:8], onesf.t[:, 0:1], pm.t[:, :], True, True, [onesf, pm], [pdn], True)
        dve(lambda e, pdn=pdn: e.tensor_reduce(out=dnm.t[:1, 0:4], in_=pdn.t[:1, 0:8].rearrange("p (m h) -> p h m", m=2), axis=AX.X, op=ALU.add), [pdn], [dnm])
        dve(lambda e: e.reciprocal(out=dnm.t[:1, 0:4], in_=dnm.t[:1, 0:4]), [dnm], [dnm])
        pos = [nb(), nb()]
        for h in range(4):
            for m in range(2):
                mm(pos[h // 2].t[:1, (h % 2) * 256:(h % 2 + 1) * 256], pm.t[:, m * 4 + h: m * 4 + h + 1], cmv_b.t[:, m, h * 256:(h + 1) * 256],
                   m == 0, m == 1, [pm, cmv_b], [pos[h // 2]], m == 1)
        for hp in range(2):
            dve(lambda e, hp=hp, pos=pos: e.tensor_tensor(out=tmpq.t[:1, hp * 512:(hp + 1) * 512].rearrange("p (h d) -> p h d", h=2),
                                                          in0=pos[hp].t[:1, :].rearrange("p (h d) -> p h d", h=2),
                                                          in1=dnm.t[:1, 2 * hp:2 * hp + 2].unsqueeze(2).to_broadcast([1, 2, 256]), op=ALU.mult),
                [pos[hp], dnm], [tmpq])
        S.dma(SP, omtm.t[b:b + 1, :], tmpq.t[0:1, :], reads=[tmpq], writes=[omtm], sem="omt")
    if dbg:
        S.dma(SP, d_om, omtm.t[:NS, :], reads=[omtm], sem="dbg")
    tm_to_fm(omtm, 8, attnT)
    proj_tm(attnT, 8, w_mo, 0, 1024, add_into_xs)
    if dbg:
        S.dma(SP, d_x2, xs_sb.t[:NS, 0, :], reads=[xs_sb], sem="dbg")
    norm_T(xs_sb, 1, NS, gffnT, hT)
    proj_tm(hT, 8, w_gate, 0, DFF, lambda c0, ncol, pb: act(g_tm.t[:, c0:c0 + ncol], pb.t[:NS, :ncol], AF.Silu, [pb], [g_tm]))

    def cons_up(c0, ncol, pb):
        dve(lambda e: e.tensor_tensor(out=g_tm.t[:, c0:c0 + ncol], in0=pb.t[:NS, :ncol], in1=g_tm.t[:, c0:c0 + ncol], op=ALU.mult), [pb, g_tm], [g_tm])
    proj_tm(hT, 8, w_up, 0, DFF, cons_up)
    tm_to_fm(g_tm, 22, fT)
    proj_tm(fT, 22, w_down, 0, 1024, add_into_xs)
    S.dma(SP, gfin.t[:, :], g_final.partition_broadcast(128), writes=[gfin], sem="const")
    rstd_only(xs_sb, 1, NS)
    dve(lambda e: e.scalar_tensor_tensor(out=ysb.t[:NS, :], in0=xs_sb.t[:NS, 0, :], scalar=rstd.t[:NS, 0:1], in1=gfin.t[:NS, :],
                                         op0=ALU.mult, op1=ALU.mult), [xs_sb, rstd, gfin], [ysb])
    S.dma(SP, o_ys, ysb.t[:NS, :], reads=[ysb], sem="o_y")

    S.finish(SP)
    S.replay(ctx)
    ctx.close()
    return nc


_NC_CACHE = {}
_WITH_SAMPLE = [True]
_TRACE = [False]


def kernel(**inp):
    f32 = np.float32
    xp = np.asarray(inp["x_prompt"], f32)
    B = xp.shape[0]
    if "nc" not in _NC_CACHE:
        _NC_CACHE["nc"] = build_program(with_sample=_WITH_SAMPLE[0])
    nc = _NC_CACHE["nc"]
    in_maps = []
    shared = {}
    for name in ("w_in", "conv_w", "w_conv_out", "w_attn_out", "w_o", "w_mq", "w_mk", "w_mv", "w_mo", "w_gate", "w_up", "w_down",
                 "g_mix", "g_mem", "g_mem_kv", "g_ffn"):
        shared[name] = np.ascontiguousarray(np.asarray(inp[name], f32)[0])
    shared["g_final"] = np.ascontiguousarray(np.asarray(inp["g_final"], f32))
    ck_ = np.ascontiguousarray(np.asarray(inp["cache_k"], f32)[0]).reshape(2560 * 8, 4096)
    cv_ = np.ascontiguousarray(np.asarray(inp["cache_v"], f32)[0]).reshape(2560 * 8, 4096)
    cik_ = np.ascontiguousarray(np.asarray(inp["cache_idx_k"], f32)[0]).reshape(2560 * 8, 1024)
    for c in range(8):
        b, j = c // 4, c % 4
        segs = [4 * t + j for t in range(NT)]
        x_own = np.concatenate([xp[b, 512 * s:512 * (s + 1)] for s in segs], axis=0)
        x_halo = np.zeros((8, D), f32)
        for t, s in enumerate(segs):
            if s > 0:
                x_halo[2 * t:2 * t + 2] = xp[b, 512 * s - 2:512 * s]
        qrel = (512.0 * j + np.arange(128, dtype=f32)).reshape(128, 1).astype(f32)
        m = dict(shared)
        m.update({"x_all": np.ascontiguousarray(xp[b]), "x_own": np.ascontiguousarray(x_own), "x_halo": x_halo, "qrel": qrel,
                  "mem": np.ascontiguousarray(np.asarray(inp["mem_prompt"], f32)[b])})
        sl = slice(16 * c, 16 * (c + 1))
        if _WITH_SAMPLE[0]:
          m.update({"xs": np.ascontiguousarray(np.asarray(inp["x_sample"], f32)[sl, 0]),
                  "sconv": np.ascontiguousarray(np.asarray(inp["state_conv"], f32)[0, sl]),
                  "ptab": np.ascontiguousarray(np.asarray(inp["page_table"], np.int32)[sl]),
                  "cache_k": ck_, "cache_v": cv_, "cache_ik": cik_,
                  "cmk": np.ascontiguousarray(np.asarray(inp["cache_mem_k"], f32)[0, sl].reshape(16, 256, D)),
                  "cmv": np.ascontiguousarray(np.asarray(inp["cache_mem_v"], f32)[0, sl].reshape(16, 256, D))})
        in_maps.append(m)
    if _TRACE[0]:
        res = run_bass_kernel_spmd(nc, in_maps, core_ids=list(range(8)), trace=True)
        print("exec_time_ns", res.exec_time_ns)
    else:
        res = run_bass_kernel_spmd(nc, in_maps, core_ids=list(range(8)))
    R = res.results
    y = np.zeros((B, SEQ, D), f32)
    nk = np.zeros((1, B, SEQ, 2, 128), f32)
    nv = np.zeros((1, B, SEQ, 2, 128), f32)
    nik = np.zeros((1, B, SEQ, 64), f32)
    nconv = np.zeros((1, B, 2, D), f32)
    nmk = np.zeros((1, B, 256, 4, 256), f32)
    nmv = np.zeros((1, B, 256, 4, 256), f32)
    for c in range(8):
        b, j = c // 4, c % 4
        for t in range(NT):
            s = 4 * t + j
            sl = slice(512 * s, 512 * (s + 1))
            tl = slice(512 * t, 512 * (t + 1))
            y[b, sl] = R[c]["o_y"][tl]
            nk[0, b, sl] = R[c]["o_k"][tl].reshape(512, 2, 128)
            nv[0, b, sl] = R[c]["o_v"][tl].reshape(512, 2, 128)
            nik[0, b, sl] = R[c]["o_ik"][tl]
        if j == 3:
            nconv[0, b] = R[c]["o_conv"]
        if j == 0:
            nmk[0, b] = R[c]["o_mk"].reshape(256, 4, 256)
            nmv[0, b] = R[c]["o_mv"].reshape(256, 4, 256)
    if not _WITH_SAMPLE[0]:
        return (y, nk, nv, nik, nconv, nmk, nmv)
    ys = np.concatenate([R[c]["o_ys"] for c in range(8)], axis=0).reshape(128, 1, D)
    nks = np.concatenate([R[c]["o_ks"] for c in range(8)], axis=0).reshape(1, 128, 1, 2, 128)
    nvs = np.concatenate([R[c]["o_vs"] for c in range(8)], axis=0).reshape(1, 128, 1, 2, 128)
    niks = np.concatenate([R[c]["o_iks"] for c in range(8)], axis=0).reshape(1, 128, 1, 64)
    ncs = np.concatenate([R[c]["o_convs"] for c in range(8)], axis=0).reshape(1, 128, 2, D)
    return (y, ys, nk, nv, nik, nconv, nmk, nmv, nks, nvs, niks, ncs)
```
